# Optimizing a Trainium2 kernel written in Bass

```python
import jax, jax.numpy as jnp
from jax import lax
import numpy as np

D_MODEL = 1024
BATCH = 16
SEQ = 2048
DEPTH = 1

N_MEM = 256
M_HEADS = 4
M_HEAD_DIM = 128
M_WIDTH = M_HEADS * M_HEAD_DIM
M_CONV = 4
M_CHUNK = 64
A_HEADS = 8
A_NOPE = 64
A_ROPE = 32
A_QK = A_NOPE + A_ROPE
A_VDIM = 64
A_WIDTH = A_HEADS * A_VDIM
A_Q_RANK = 256
A_KV_RANK = 128
ROPE_THETA = 10000.0
Q_BLOCK = 128
X_HEADS = 4
X_HEAD_DIM = D_MODEL // X_HEADS
N_EXPERTS = 32
TOP_K = 4
D_EXPERT = D_MODEL
SWIGLU_ALPHA = 1.702
SWIGLU_LIMIT = 7.0
MOE_BLOCK = 128
DN_ALPHA = (2.0 * DEPTH) ** 0.25
DN_BETA = (8.0 * DEPTH) ** -0.25
EPS = 1e-5
IN_SPLITS = (A_Q_RANK, A_KV_RANK, A_ROPE, M_WIDTH, M_WIDTH, M_WIDTH, M_HEADS, M_HEADS, D_MODEL, D_MODEL)
IN_OFFSETS = tuple(int(v) for v in np.cumsum(IN_SPLITS)[:-1])
D_IN = int(sum(IN_SPLITS))

kernel_name = 'hybrid_mlstm_mla_gated_moe_deepnorm'


def layer_norm(x, g, b):
    xf = x.astype(jnp.float32)
    mu = xf.mean(-1, keepdims=True)
    var = jnp.square(xf - mu).mean(-1, keepdims=True)
    return ((xf - mu) * lax.rsqrt(var + EPS) * g + b).astype(x.dtype)


def rms_norm(x, g):
    xf = x.astype(jnp.float32)
    return (xf * lax.rsqrt(jnp.square(xf).mean(-1, keepdims=True) + EPS) * g).astype(x.dtype)


def rope_cos_sin(positions):
    half = A_ROPE // 2
    inv_freq = ROPE_THETA ** (-jnp.arange(half, dtype=jnp.float32) / half)
    ang = positions.astype(jnp.float32)[..., None] * inv_freq
    return jnp.cos(ang), jnp.sin(ang)


def apply_rope(x, cos, sin):
    x1, x2 = jnp.split(x.astype(jnp.float32), 2, axis=-1)
    return jnp.concatenate([x1 * cos - x2 * sin, x2 * cos + x1 * sin], axis=-1).astype(x.dtype)


def mlstm_chunkwise(q, k, v, i_pre, f_pre):
    bsz, s, nh, d = q.shape
    nc = s // M_CHUNK
    f32 = jnp.float32

    def to_chunks(t):
        return t.astype(f32).reshape(bsz, nc, M_CHUNK, nh, -1).transpose(0, 3, 1, 2, 4)

    q, v = to_chunks(q), to_chunks(v)
    k = to_chunks(k) * d ** -0.5
    ig = to_chunks(i_pre[..., None])[..., 0]
    lf = jax.nn.log_sigmoid(to_chunks(f_pre[..., None])[..., 0])
    b = jnp.cumsum(lf, axis=-1)
    b_tot = b[..., -1]
    causal = jnp.tril(jnp.ones((M_CHUNK, M_CHUNK), dtype=bool))
    log_d = jnp.where(causal, b[..., :, None] - b[..., None, :] + ig[..., None, :], -jnp.inf)
    m_intra = log_d.max(-1)

    a = b_tot[..., None] - b + ig
    m_chunk = a.max(-1)
    wa = jnp.exp(a - m_chunk[..., None])
    g_c = jnp.einsum('bhcl,bhclv,bhclk->bhcvk', wa, v, k)
    n_c = jnp.einsum('bhcl,bhclk->bhck', wa, k)

    def step(carry, xs):
        c_st, n_st, m_st = carry
        bt, mc, gc, nvec = xs
        m_new = jnp.maximum(bt + m_st, mc)
        s_old = jnp.exp(bt + m_st - m_new)
        s_new = jnp.exp(mc - m_new)
        c_new = s_old[..., None, None] * c_st + s_new[..., None, None] * gc
        n_new = s_old[..., None] * n_st + s_new[..., None] * nvec
        return (c_new, n_new, m_new), (c_st, n_st, m_st)

    init = (jnp.zeros((bsz, nh, d, d), f32), jnp.zeros((bsz, nh, d), f32), jnp.zeros((bsz, nh), f32))
    xs = (jnp.moveaxis(b_tot, 2, 0), jnp.moveaxis(m_chunk, 2, 0), jnp.moveaxis(g_c, 2, 0), jnp.moveaxis(n_c, 2, 0))
    _, (c_prev, n_prev, m_prev) = lax.scan(step, init, xs)
    c_prev = jnp.moveaxis(c_prev, 0, 2)
    n_prev = jnp.moveaxis(n_prev, 0, 2)
    m_prev = jnp.moveaxis(m_prev, 0, 2)

    g_inter = b + m_prev[..., None]
    m = jnp.maximum(g_inter, m_intra)
    w_inter = jnp.exp(g_inter - m)
    scores = jnp.einsum('bhcjd,bhcsd->bhcjs', q, k) * jnp.exp(log_d - m[..., None])
    num = (w_inter[..., None] * jnp.einsum('bhcvk,bhcjk->bhcjv', c_prev, q)
           + jnp.einsum('bhcjs,bhcsv->bhcjv', scores, v))
    den = w_inter * jnp.einsum('bhck,bhcjk->bhcj', n_prev, q) + scores.sum(-1)
    h = num / jnp.maximum(jnp.abs(den), jnp.exp(-m))[..., None]
    return h.transpose(0, 2, 3, 1, 4).reshape(bsz, s, nh, d)


def mlstm_branch(xm, vm, o_pre, i_pre, f_pre, b_i, b_f, conv_w, conv_b, w_mq, w_mk, g_mhead, w_mskip):
    bsz, s, _ = xm.shape
    xpad = jnp.pad(xm, ((0, 0), (M_CONV - 1, 0), (0, 0)))
    xc = jax.nn.silu(sum(xpad[:, j:j + s] * conv_w[j] for j in range(M_CONV)) + conv_b)
    xch = xc.reshape(bsz, s, M_HEADS, M_HEAD_DIM)
    q = jnp.einsum('bshd,hde->bshe', xch, w_mq)
    k = jnp.einsum('bshd,hde->bshe', xch, w_mk)
    v = vm.reshape(bsz, s, M_HEADS, M_HEAD_DIM)
    h = mlstm_chunkwise(q, k, v, i_pre + b_i, f_pre + b_f)
    mu = h.mean(-1, keepdims=True)
    var = jnp.square(h - mu).mean(-1, keepdims=True)
    hn = (((h - mu) * lax.rsqrt(var + EPS)).reshape(bsz, s, M_WIDTH) * g_mhead).astype(xm.dtype)
    return jax.nn.sigmoid(o_pre) * (hn + w_mskip * xc)


def causal_block_attention(q, k, v, scale):
    bsz, s, nh, dq = q.shape
    nq = s // Q_BLOCK
    qb = q.reshape(bsz, nq, Q_BLOCK, nh, dq).swapaxes(0, 1)
    key_idx = jnp.arange(s)

    def one_block(args):
        qi, blk = args
        sc = jnp.einsum('bqhd,bkhd->bhqk', qi, k).astype(jnp.float32) * scale
        q_idx = blk * Q_BLOCK + jnp.arange(Q_BLOCK)
        sc = jnp.where(key_idx[None, :] <= q_idx[:, None], sc, -jnp.inf)
        p = jax.nn.softmax(sc, axis=-1).astype(v.dtype)
        return jnp.einsum('bhqk,bkhd->bqhd', p, v)

    o = lax.map(one_block, (qb, jnp.arange(nq)))
    return o.swapaxes(0, 1).reshape(bsz, s, nh * v.shape[-1])


def mla_branch(q_lat, kv_lat, k_rope, positions, g_qlat, g_kvlat, w_uq, w_ukv):
    bsz, s, _ = q_lat.shape
    q = (rms_norm(q_lat, g_qlat) @ w_uq).reshape(bsz, s, A_HEADS, A_QK)
    kv = (rms_norm(kv_lat, g_kvlat) @ w_ukv).reshape(bsz, s, A_HEADS, A_NOPE + A_VDIM)
    q_nope, q_pe = q[..., :A_NOPE], q[..., A_NOPE:]
    k_nope, v = kv[..., :A_NOPE], kv[..., A_NOPE:]
    cos, sin = rope_cos_sin(positions)
    q_pe = apply_rope(q_pe, cos[:, :, None, :], sin[:, :, None, :])
    k_pe = apply_rope(k_rope, cos, sin)
    q = jnp.concatenate([q_nope, q_pe], axis=-1)
    k = jnp.concatenate([k_nope, jnp.broadcast_to(k_pe[:, :, None, :], (bsz, s, A_HEADS, A_ROPE))], axis=-1)
    return causal_block_attention(q, k, v, A_QK ** -0.5)


def memory_cross_attention(x, mem, w_cq, w_ckv, w_co):
    bsz, s, _ = x.shape
    q = (x @ w_cq).reshape(bsz, s, X_HEADS, X_HEAD_DIM)
    kv = (mem @ w_ckv).reshape(bsz, mem.shape[1], 2, X_HEADS, X_HEAD_DIM)
    k, v = kv[:, :, 0], kv[:, :, 1]
    sc = jnp.einsum('bshd,bmhd->bhsm', q, k).astype(jnp.float32) * X_HEAD_DIM ** -0.5
    p = jax.nn.softmax(sc, axis=-1).astype(v.dtype)
    o = jnp.einsum('bhsm,bmhd->bshd', p, v).reshape(bsz, s, D_MODEL)
    return o @ w_co


def clamped_swiglu(gu):
    x_glu, x_lin = gu[..., ::2], gu[..., 1::2]
    x_glu = jnp.minimum(x_glu, SWIGLU_LIMIT)
    x_lin = jnp.clip(x_lin, -SWIGLU_LIMIT, SWIGLU_LIMIT)
    return x_glu * jax.nn.sigmoid(SWIGLU_ALPHA * x_glu) * (x_lin + 1.0)


def moe_ffn(x, w_router, b_router, w_gu, b_gu, w_dn, b_dn):
    bsz, s, d = x.shape
    t = bsz * s
    xt = x.reshape(t, d)
    logits = (xt @ w_router + b_router).astype(jnp.float32)
    top_val, top_idx = lax.top_k(logits, TOP_K)
    gate = jax.nn.softmax(top_val, axis=-1)
    n_assign = t * TOP_K
    e_flat = top_idx.reshape(-1).astype(jnp.int32)
    tok_flat = jnp.arange(n_assign, dtype=jnp.int32) // TOP_K
    w_flat = gate.reshape(-1)
    order = jnp.argsort(e_flat)
    e_sorted, tok_sorted, w_sorted = e_flat[order], tok_flat[order], w_flat[order]
    counts = jnp.zeros((N_EXPERTS,), jnp.int32).at[e_flat].add(1)
    padded = (counts + MOE_BLOCK - 1) // MOE_BLOCK * MOE_BLOCK
    starts = jnp.cumsum(counts) - counts
    pends = jnp.cumsum(padded)
    pstarts = pends - padded
    dest = pstarts[e_sorted] + (jnp.arange(n_assign, dtype=jnp.int32) - starts[e_sorted])
    n_rows = n_assign + N_EXPERTS * MOE_BLOCK
    n_blocks = n_rows // MOE_BLOCK
    buf_tok = jnp.full((n_rows,), t, jnp.int32).at[dest].set(tok_sorted)
    buf_w = jnp.zeros((n_rows,), jnp.float32).at[dest].set(w_sorted)
    blk_e = jnp.minimum(jnp.searchsorted(pends, jnp.arange(n_blocks, dtype=jnp.int32) * MOE_BLOCK, side='right'),
                        N_EXPERTS - 1)
    x_pad = jnp.concatenate([xt, jnp.zeros((1, d), xt.dtype)], axis=0)
    xb = x_pad[buf_tok].reshape(n_blocks, MOE_BLOCK, d)

    def expert_block(args):
        xi, e = args
        return clamped_swiglu(xi @ w_gu[e] + b_gu[e]) @ w_dn[e] + b_dn[e]

    yb = lax.map(expert_block, (xb, blk_e)).reshape(n_rows, d)
    y = jnp.zeros((t + 1, d), x.dtype).at[buf_tok].add(yb * buf_w[:, None].astype(yb.dtype))
    return y[:t].reshape(bsz, s, d)


def setup_inputs(seed: int = 0) -> dict:
    key = jax.random.key(seed)
    ks = iter(jax.random.split(key, 48))
    L, D = DEPTH, D_MODEL

    def nrm(shape, scale):
        return scale * jax.random.normal(next(ks), shape, jnp.float32)

    def gain(shape):
        return 1.0 + nrm(shape, 0.02)

    offsets = jax.random.randint(next(ks), (BATCH, 1), 0, 4096, dtype=jnp.int32)
    return {
        'x': nrm((BATCH, SEQ, D), 1.0),
        'mem': nrm((BATCH, N_MEM, D), 1.0),
        'positions': (offsets + jnp.arange(SEQ, dtype=jnp.int32)[None, :]).astype(jnp.int32),
        'w_in': nrm((L, D, D_IN), D ** -0.5),
        'b_igate': nrm((L, M_HEADS), 0.1),
        'b_fgate': jnp.linspace(3.0, 6.0, M_HEADS, dtype=jnp.float32)[None, :] + nrm((L, M_HEADS), 0.1),
        'conv_w': nrm((L, M_CONV, M_WIDTH), M_CONV ** -0.5),
        'conv_b': nrm((L, M_WIDTH), 0.01),
        'w_mq': nrm((L, M_HEADS, M_HEAD_DIM, M_HEAD_DIM), M_HEAD_DIM ** -0.5),
        'w_mk': nrm((L, M_HEADS, M_HEAD_DIM, M_HEAD_DIM), M_HEAD_DIM ** -0.5),
        'g_mhead': gain((L, M_WIDTH)),
        'w_mskip': gain((L, M_WIDTH)),
        'g_qlat': gain((L, A_Q_RANK)),
        'g_kvlat': gain((L, A_KV_RANK)),
        'w_uq': nrm((L, A_Q_RANK, A_HEADS * A_QK), A_Q_RANK ** -0.5),
        'w_ukv': nrm((L, A_KV_RANK, A_HEADS * (A_NOPE + A_VDIM)), A_KV_RANK ** -0.5),
        'w_br_m': nrm((L, M_WIDTH, D), M_WIDTH ** -0.5),
        'w_br_a': nrm((L, A_WIDTH, D), A_WIDTH ** -0.5),
        'w_mix_out': nrm((L, D, D), DN_BETA * D ** -0.5),
        'ln1_g': gain((L, D)),
        'ln1_b': nrm((L, D), 0.01),
        'w_cq': nrm((L, D, D), D ** -0.5),
        'w_ckv': nrm((L, D, 2 * D), D ** -0.5),
        'w_co': nrm((L, D, D), DN_BETA * D ** -0.5),
        'ln2_g': gain((L, D)),
        'ln2_b': nrm((L, D), 0.01),
        'w_router': nrm((L, D, N_EXPERTS), D ** -0.5),
        'b_router': nrm((L, N_EXPERTS), 0.01),
        'w_gu': nrm((L, N_EXPERTS, D, 2 * D_EXPERT), D ** -0.5),
        'b_gu': nrm((L, N_EXPERTS, 2 * D_EXPERT), 0.01),
        'w_dn': nrm((L, N_EXPERTS, D_EXPERT, D), DN_BETA * D_EXPERT ** -0.5),
        'b_dn': nrm((L, N_EXPERTS, D), 0.01),
        'ln3_g': gain((L, D)),
        'ln3_b': nrm((L, D), 0.01),
    }


def reference(x, mem, positions, w_in, b_igate, b_fgate, conv_w, conv_b, w_mq, w_mk, g_mhead, w_mskip,
              g_qlat, g_kvlat, w_uq, w_ukv, w_br_m, w_br_a, w_mix_out, ln1_g, ln1_b,
              w_cq, w_ckv, w_co, ln2_g, ln2_b, w_router, b_router, w_gu, b_gu, w_dn, b_dn, ln3_g, ln3_b):
    h = x
    for l in range(DEPTH):
        proj = h @ w_in[l]
        q_lat, kv_lat, k_rope, xm, vm, o_pre, i_pre, f_pre, g_m, g_a = jnp.split(proj, IN_OFFSETS, axis=-1)
        y_m = mlstm_branch(xm, vm, o_pre, i_pre, f_pre, b_igate[l], b_fgate[l], conv_w[l], conv_b[l],
                           w_mq[l], w_mk[l], g_mhead[l], w_mskip[l]) @ w_br_m[l]
        y_a = mla_branch(q_lat, kv_lat, k_rope, positions, g_qlat[l], g_kvlat[l], w_uq[l], w_ukv[l]) @ w_br_a[l]
        mix = (jax.nn.sigmoid(g_m) * y_m + jax.nn.sigmoid(g_a) * y_a) @ w_mix_out[l]
        h = layer_norm(DN_ALPHA * h + mix, ln1_g[l], ln1_b[l])
        h = layer_norm(DN_ALPHA * h + memory_cross_attention(h, mem, w_cq[l], w_ckv[l], w_co[l]), ln2_g[l], ln2_b[l])
        h = layer_norm(DN_ALPHA * h + moe_ffn(h, w_router[l], b_router[l], w_gu[l], b_gu[l], w_dn[l], b_dn[l]),
                       ln3_g[l], ln3_b[l])
    return h
```

```python
import contextlib
import os
import numpy as np
import concourse.bass as bass
import concourse.mybir as mybir
from concourse.bass_utils import run_bass_kernel_spmd

F32 = mybir.dt.float32
BF16 = mybir.dt.bfloat16
I32 = mybir.dt.int32
AF = mybir.ActivationFunctionType
ALU = mybir.AluOpType

ENGS = ("pe", "act", "dve", "pool", "sp")

N_CORES = 8
NB = 2
S = 2048
T = NB * S
D = 1024
NG = 4
CAP = 640
NE = 32
NSLOT = NE * CAP
DN_ALPHA = 2.0 ** 0.25
EPS = 1e-5
BIG = 1.0e6


class _Op:
    __slots__ = ("eng", "fn", "deps", "is_dma", "tile", "dma_val", "signal", "sig_val")


class Prog:
    def __init__(self, nc):
        self.nc = nc
        self.ops = {e: [] for e in ENGS}
        self.last_w = {}
        self.readers = {}
        self.dma_cnt = {}
        self.dma_tiles = []
        self.n = 0
        self.limit = int(os.environ.get("KCUT", "0")) or None

    def mark(self, label):
        if os.environ.get("KDBG"):
            print("MARK", label, self.n)

    def op(self, eng, fn, reads=(), writes=(), dma=False):
        self.n += 1
        if self.limit is not None and self.n > self.limit and fn is not None:
            fn = None
            dma = False
            reads, writes = (), ()
        reads = [k[:3] if k.startswith("ps") else k for k in reads]
        writes = [k[:3] if k.startswith("ps") else k for k in writes]
        writes = writes + [k for k in reads if k.startswith("ps") and k not in writes]
        o = _Op()
        o.eng, o.fn, o.is_dma, o.signal, o.sig_val = eng, fn, dma, False, 0
        deps = []
        for t in reads:
            w = self.last_w.get(t)
            if w is not None:
                deps.append(w)
        for t in writes:
            w = self.last_w.get(t)
            if w is not None and not (dma and w.is_dma and w.tile == writes[0]):
                deps.append(w)
            deps.extend(self.readers.get(t, ()))
        if dma:
            t0 = writes[0]
            if t0 not in self.dma_cnt:
                self.dma_cnt[t0] = 0
                self.dma_tiles.append(t0)
            self.dma_cnt[t0] += 1
            o.tile, o.dma_val = t0, 16 * self.dma_cnt[t0]
        else:
            o.tile, o.dma_val = None, 0
        dd = []
        for d in deps:
            if d is o:
                continue
            if (not d.is_dma) and (not dma) and d.eng == eng == "pe":
                continue
            dd.append(d)
        o.deps = dd
        for t in reads:
            self.readers.setdefault(t, []).append(o)
        for t in writes:
            self.last_w[t] = o
            self.readers[t] = []
        self.ops[eng].append(o)
        return o

    def barrier_all(self):
        lasts = []
        for e in ENGS:
            comp = [o for o in self.ops[e] if not o.is_dma and o.fn is not None]
            if comp:
                lasts.append(comp[-1])
        dmas = {}
        for e in ENGS:
            for o in self.ops[e]:
                if o.is_dma:
                    dmas[o.tile] = o
        lasts.extend(dmas.values())
        for e in ENGS:
            o = self.op(e, None)
            o.deps = list(lasts)
        self.last_w = {}
        self.readers = {}

    def emit(self, final_tiles=()):
        nc = self.nc
        fin = self.op("sp", None)
        fin.deps = [self.last_w[t] for t in final_tiles if t in self.last_w]
        for e in ENGS:
            comp = [o for o in self.ops[e] if not o.is_dma and o.fn is not None]
            if comp:
                fin.deps.append(comp[-1])
        for e in ENGS:
            for o in self.ops[e]:
                for d in o.deps:
                    if not d.is_dma:
                        d.signal = True
        for e in ENGS:
            c = 0
            for o in self.ops[e]:
                if o.signal and not o.is_dma:
                    c += 1
                    o.sig_val = c
        with contextlib.ExitStack() as st:
            esem = {e: st.enter_context(nc.semaphore("s_" + e)) for e in ENGS}
            dsem = {}
            for i, t in enumerate(self.dma_tiles):
                dsem[t] = st.enter_context(nc.semaphore("d%d" % i))
            block = st.enter_context(nc.Block())

            def run(e):
                def body(eng):
                    known = {}
                    for o in self.ops[e]:
                        need = {}
                        for d in o.deps:
                            if d.is_dma:
                                key, val = ("d", d.tile), d.dma_val
                            else:
                                key, val = ("e", d.eng), d.sig_val
                            if val > need.get(key, 0):
                                need[key] = val
                        for key, val in need.items():
                            if known.get(key, 0) >= val:
                                continue
                            known[key] = val
                            sem = dsem[key[1]] if key[0] == "d" else esem[key[1]]
                            eng.wait_ge(sem, val)
                        if o.fn is None:
                            continue
                        ins = o.fn(eng)
                        if o.is_dma:
                            ins.then_inc(dsem[o.tile], 16)
                        elif o.signal:
                            ins.then_inc(esem[e], 1)
                return body

            block.tensor(run("pe"))
            block.scalar(run("act"))
            block.vector(run("dve"))
            block.gpsimd(run("pool"))
            block.sync(run("sp"))


class Arena:
    def __init__(self, tensor, nbytes):
        self.t, self.n, self.off = tensor, nbytes, 0

    def alloc(self, free_shape, dt, parts=128):
        esz = 2 if dt == BF16 else 4
        n = 1
        for s_ in free_shape:
            n *= s_
        nb = (n * esz + 63) // 64 * 64
        if self.off + nb > self.n:
            raise RuntimeError("arena overflow: need %d at %d of %d" % (nb, self.off, self.n))
        o = self.off // 2
        self.off += nb
        ap = self.t[:, o:o + n * esz // 2]
        if dt != BF16:
            ap = ap.bitcast(dt)
        if len(free_shape) == 2:
            ap = ap.rearrange("p (a b) -> p a b", a=free_shape[0])
        elif len(free_shape) == 3:
            ap = ap.rearrange("p (a b c) -> p a b c", a=free_shape[0], b=free_shape[1])
        if parts != 128:
            ap = ap[0:parts]
        return ap

    def mark(self):
        return self.off

    def release(self, m):
        self.off = m


def build_program(stage="full", debug=False, skip1=False):
    nc = bass.Bass("TRN2", target_bir_lowering=False)

    def din(name, shape, dt=F32):
        return nc.dram_tensor(name, list(shape), dt, kind="ExternalInput").ap()

    def dscr(name, shape, dt, dbg=False):
        kind = "ExternalOutput" if (debug and dbg) else "Internal"
        return nc.dram_tensor(name, list(shape), dt, kind=kind).ap()

    x_d = din("x", [T, D])
    mem_d = din("mem", [NB * 256, D])
    pos_d = din("pos", [NB, S], I32)
    wa_d = din("w_a", [D, 512])
    wb_d = din("w_b", [D, 1544])
    wc_d = din("w_c", [D, 2048])
    wuq_d = din("w_uq2", [256, 1536])
    wukv_d = din("w_ukv2", [128, 1024])
    wmqk_d = din("w_mqk", [128, 1024])
    pm_d = din("pm", [128, 32])
    gb_d = din("gb", [1, 8])
    lnp_d = din("lnp", [6, D])
    wbrm_d = din("w_br_m", [512, D])
    wbra_d = din("w_br_a", [512, D])
    wmix_d = din("w_mix", [D, D])
    wcq_d = din("w_cq", [D, D])
    wckv_d = din("w_ckv", [D, 2 * D])
    wco_d = din("w_co", [D, D])
    wr_d = din("w_router", [D, NE])
    br_d = din("b_router", [1, NE])
    wglu_d = din("w_glu", [NE, D, D])
    wlin_d = din("w_lin", [NE, D, D])
    wdn_d = din("w_dn", [NE, D, D])
    bglu_d = din("b_glu", [NE, D])
    blin_d = din("b_lin", [NE, D])
    bdn_d = din("b_dn", [NE, D])
    out_d = nc.dram_tensor("out", [T, D], F32, kind="ExternalOutput").ap()

    xT_d = dscr("xT_scr", [NB * NG, 128, 8 * 512], BF16)
    h1_d = din("h1_in", [T, D]) if skip1 else dscr("h1_scr", [T, D], F32, True)
    h2_d = dscr("h2_scr", [T, D], F32, True)
    dbg_d = dscr("dbg_scr", [128, 8 * S], BF16, True)
    dbg2_d = dscr("dbg2_scr", [128, 256], F32, True)
    xs_d = dscr("xs_scr", [NSLOT, D], BF16)
    ys_d = dscr("ys_scr", [NSLOT, D], BF16)

    ARENA_BYTES = 204 * 1024
    arena_t = nc.alloc_sbuf_tensor("arena", [128, ARENA_BYTES // 2], BF16)
    A = Arena(arena_t, ARENA_BYTES)
    psb = [nc.alloc_psum_tensor("ps%d" % i, [128, 512], F32) for i in range(8)]

    def PS(i, n=512, parts=128, c0=0):
        return psb[i][0:parts, c0:c0 + n]

    def PSB(i, n=1024, parts=128, c0=0):
        return psb[i][:, :].bitcast(BF16)[0:parts, c0:c0 + n]

    P = Prog(nc)
    _bc = {}

    def bc_reg(e):
        if "r" not in _bc:
            _bc["r"] = e.to_reg(NSLOT - 1)
        return _bc["r"]

    def OP(eng, method, reads, writes, *args, **kw):
        return P.op(eng, lambda e: getattr(e, method)(*args, **kw), reads=reads, writes=writes)

    def DMA(q, out, in_, reads, writes, **kw):
        return P.op(q, lambda e: e.dma_start(out=out, in_=in_, **kw), reads=reads, writes=writes, dma=True)

    def MM(out, lhsT, rhs, start, stop, reads, writes):
        return P.op("pe", lambda e: e.matmul(out, lhsT=lhsT, rhs=rhs, start=start, stop=stop),
                    reads=reads, writes=writes)

    def ACT(out, in_, func, reads, writes, **kw):
        return P.op("act", lambda e: e.activation(out=out, in_=in_, func=func, **kw), reads=reads, writes=writes)

    iota_f = A.alloc([512], F32)
    pidx = A.alloc([1], F32)
    tri_f = A.alloc([128], F32)
    stri_b = A.alloc([128], BF16)
    ident_b = A.alloc([128], BF16)
    ident_f = A.alloc([128], F32)
    ones_f = A.alloc([128], F32)
    ones_b = A.alloc([128], BF16)
    masks = A.alloc([4, 512], BF16)
    eps_c = A.alloc([1], F32)
    one_c = A.alloc([1], F32)
    pm = A.alloc([32], F32)
    gbt = A.alloc([8], F32)
    ropec = A.alloc([4], F32)
    CONST = ["const"]

    OP("pool", "iota", [], ["iota"], iota_f, [[1, 512]], base=0, channel_multiplier=-1,
       allow_small_or_imprecise_dtypes=True)
    OP("pool", "iota", [], ["pidx"], pidx, [[0, 1]], base=0, channel_multiplier=1,
       allow_small_or_imprecise_dtypes=True)
    OP("dve", "tensor_single_scalar", ["iota"], ["c1"], out=tri_f, in_=iota_f[:, 0:128], scalar=0.0, op=ALU.is_ge)
    OP("dve", "tensor_single_scalar", ["iota"], ["c2"], out=stri_b, in_=iota_f[:, 0:128], scalar=1.0, op=ALU.is_ge)
    OP("dve", "tensor_single_scalar", ["iota"], ["c3"], out=ident_b, in_=iota_f[:, 0:128], scalar=0.0, op=ALU.is_equal)
    OP("dve", "tensor_single_scalar", ["iota"], ["c4"], out=ident_f, in_=iota_f[:, 0:128], scalar=0.0, op=ALU.is_equal)
    for j in range(4):
        OP("dve", "tensor_single_scalar", ["iota"], ["c5%d" % j], out=masks[:, j, :], in_=iota_f,
           scalar=float(128 * j), op=ALU.is_ge)
    OP("pool", "memset", [], ["c6"], ones_f, 1.0)
    OP("pool", "memset", [], ["c7"], ones_b, 1.0)
    OP("pool", "memset", [], ["c8"], eps_c, EPS)
    OP("pool", "memset", [], ["c9"], one_c, 1.0)
    DMA("sp", pm, pm_d, [], ["pm"])
    DMA("sp", gbt, gb_d[0:1, :].broadcast_to([128, 8]), [], ["gbt"])
    OP("dve", "tensor_single_scalar", ["pidx"], ["rc2"], out=ropec[:, 2:3], in_=pidx, scalar=80.0, op=ALU.is_ge)
    OP("dve", "scalar_tensor_tensor", ["rc2", "pidx"], ["rc3"], out=ropec[:, 3:4], in0=ropec[:, 2:3], scalar=-16.0,
       in1=pidx, op0=ALU.mult, op1=ALU.add)
    OP("dve", "tensor_scalar", ["rc3"], ["rc3b"], out=ropec[:, 3:4], in0=ropec[:, 3:4], scalar1=-64.0, scalar2=None,
       op0=ALU.add)
    ACT(ropec[:, 0:1], ropec[:, 3:4], AF.Exp, ["rc3b"], ["rc0"], scale=-float(np.log(10000.0)) / 16.0)
    OP("dve", "tensor_scalar", ["rc0"], ["rc0b"], out=ropec[:, 0:1], in0=ropec[:, 0:1],
       scalar1=float(1.0 / (2.0 * np.pi)), scalar2=None, op0=ALU.mult)
    OP("dve", "tensor_scalar", ["rc2"], ["rc1"], out=ropec[:, 1:2], in0=ropec[:, 2:3], scalar1=2.0, scalar2=-1.0,
       op0=ALU.mult, op1=ALU.add)
    P.barrier_all()
    base_mark = A.mark()

    def rstd_from_var(var_ap, out_ap, tmp_ap, rk, wk):
        ACT(tmp_ap, var_ap, AF.Ln, rk, [wk + "_t"], bias=eps_c[0:var_ap.shape[0]])
        ACT(out_ap, tmp_ap, AF.Exp, [wk + "_t"], [wk], scale=-0.5)

    def layer_norm_tile(xres, psl, psr, lng, lnb, pre, outt, st12, mv, tmpc, key, rk_res, rk_ps):
        kp, ks = key + "pre", key + "sm"
        OP("dve", "scalar_tensor_tensor", rk_res + [rk_ps[0]], [kp], out=pre[:, 0:512], in0=xres[:, 0:512],
           scalar=DN_ALPHA, in1=psl, op0=ALU.mult, op1=ALU.add)
        OP("dve", "scalar_tensor_tensor", rk_res + [rk_ps[1], kp], [kp], out=pre[:, 512:1024], in0=xres[:, 512:1024],
           scalar=DN_ALPHA, in1=psr, op0=ALU.mult, op1=ALU.add)
        OP("dve", "bn_stats", [kp], [ks], out=st12[:, 0:6], in_=pre[:, 0:512])
        OP("dve", "bn_stats", [kp, ks], [ks], out=st12[:, 6:12], in_=pre[:, 512:1024])
        OP("dve", "bn_aggr", [ks], [ks], out=mv[:, 0:2], in_=st12[:, 0:12])
        ACT(tmpc, mv[:, 1:2], AF.Ln, [ks], [ks], bias=eps_c)
        ACT(mv[:, 2:3], tmpc, AF.Exp, [ks], [ks], scale=-0.5)
        OP("dve", "tensor_scalar", [kp, ks], [kp], out=pre, in0=pre,
           scalar1=mv[:, 0:1], scalar2=mv[:, 2:3], op0=ALU.subtract, op1=ALU.mult)
        OP("pool", "tensor_tensor", [kp, "lnp"], [kp], out=pre, in0=pre, in1=lng, op=ALU.mult)
        OP("pool", "tensor_tensor", [kp, "lnp"], [key + "out"], out=outt, in0=pre, in1=lnb, op=ALU.add)

    U = A.alloc([32768], BF16)
    attnT = A.alloc([4, S], BF16)
    ymT = A.alloc([4, S], BF16)
    p1_mark = A.mark()
    SC = float(96.0 ** -0.5)

    for b in range(0 if skip1 else NB):
        A.release(p1_mark)
        KT = U[:, 0:16384].rearrange("p (h s) -> p h s", h=8)
        VA = U[:, 16384:32768].rearrange("p (t h c) -> p t h c", t=16, h=8)
        w_a = A.alloc([8, 512], BF16)
        w_uq = A.alloc([2, 1536], BF16)
        w_ukv = A.alloc([1024], BF16)
        for k in range(8):
            DMA("pool", w_a[:, k, :], wa_d[k * 128:(k + 1) * 128, :], [], ["w_a"])
        for k in range(2):
            DMA("pool", w_uq[:, k, :], wuq_d[k * 128:(k + 1) * 128, :], [], ["w_uq"])
        DMA("pool", w_ukv, wukv_d, [], ["w_ukv"])
        xf = A.alloc([2, 1024], F32)
        xb = A.alloc([2, 1024], BF16)
        xT = A.alloc([8, 512], BF16)
        sq = A.alloc([3, 512], F32)
        lat = A.alloc([3, 512], F32)
        rsq = A.alloc([2, 512], F32)
        latn = A.alloc([3, 512], BF16)
        posi = A.alloc([512], I32)
        rp = A.alloc([6, 512], F32)
        rt = A.alloc([2, 512], F32)
        kpe = A.alloc([512], BF16)
        qT = A.alloc([8, 512], BF16)
        PT = A.alloc([3, 512], BF16)
        dsb = A.alloc([2, 512], F32)
        OP("pool", "memset", [], ["VA%d" % t for t in range(16)], U[:, 16384:32768], 1.0)
        if b == 0:
            zt = A.alloc([4, 1024], BF16)
            OP("pool", "memset", [], ["zt"], zt, 0.0)
            xs_v = xs_d.rearrange("(n j p) d -> n p j d", p=128, j=4)
            for n in range(NSLOT // 512):
                DMA("sp", xs_v[n], zt, ["zt"], ["xs_zero"])
        for g in range(NG):
            G = b * NG + g
            gs = slice(g * 512, (g + 1) * 512)
            for t in range(4):
                tt = G * 4 + t
                s2 = t % 2
                DMA("sp", xf[:, s2, :], x_d[tt * 128:(tt + 1) * 128, :], [], ["xf%d" % s2])
                ACT(xb[:, s2, :], xf[:, s2, :], AF.Copy, ["xf%d" % s2], ["xb%d" % s2])
                for k in range(8):
                    OP("pe", "transpose", ["xb%d" % s2], ["ps%d" % (6 + s2)], out=PSB(6 + s2, 128, c0=k * 128),
                       in_=xb[:, s2, k * 128:(k + 1) * 128], identity=ident_b)
                OP("dve", "tensor_copy", ["ps%d" % (6 + s2)], ["xT"], out=xT[:, :, t * 128:(t + 1) * 128],
                   in_=PSB(6 + s2).rearrange("p (k c) -> p k c", k=8))
            DMA("sp", xT_d[G].rearrange("p (k c) -> p k c", k=8), xT, ["xT"], ["xT_d%d" % G])
            P.mark('lat g%d' % g)
            for c in range(3):
                pb = c % 2
                for k in range(8):
                    MM(PS(pb), w_a[:, k, c * 128:(c + 1) * 128], xT[:, k, :], k == 0, k == 7, ["w_a", "xT"], ["ps%d" % pb])
                ACT(sq[:, c, :], PS(pb), AF.Square, ["ps%d" % pb], ["sq%d" % c])
                gcol = pm[:, 28 + c:29 + c]
                OP("dve", "tensor_scalar", ["ps%d" % pb, "pm"], ["lat%d" % c], out=lat[:, c, :], in0=PS(pb), scalar1=gcol,
                   scalar2=None, op0=ALU.mult)
            MM(PS(2), ones_f, sq[:, 0, :], True, False, ["sq0"], ["ps2"])
            MM(PS(2), ones_f, sq[:, 1, :], False, True, ["sq1"], ["ps2"])
            MM(PS(3), ones_f, sq[:, 2, :], True, True, ["sq2"], ["ps3"])
            for i_, (bank, nrm) in enumerate(((2, 1.0 / 256.0), (3, 1.0 / 128.0))):
                ACT(rsq[:, i_, :], PS(bank), AF.Ln, ["ps%d" % bank], ["rsq%d" % i_], scale=nrm, bias=eps_c)
                ACT(rsq[:, i_, :], rsq[:, i_, :], AF.Exp, ["rsq%d" % i_], ["rsq%d" % i_], scale=-0.5)
            for c in range(3):
                OP("pool", "tensor_tensor", ["lat%d" % c, "rsq%d" % (c // 2)], ["latn%d" % c], out=latn[:, c, :],
                   in0=lat[:, c, :], in1=rsq[:, c // 2, :], op=ALU.mult)
            P.mark('rope g%d' % g)
            R = slice(64, 96)
            DMA("sp", posi[R], pos_d[b, gs].partition_broadcast(32), [], ["posi"])
            OP("dve", "tensor_copy", ["posi"], ["rp0"], out=rp[R, 0, :], in_=posi[R])
            OP("dve", "tensor_scalar", ["rp0"], ["rp0"], out=rp[R, 0, :], in0=rp[R, 0, :], scalar1=ropec[R, 0:1],
               scalar2=None, op0=ALU.mult)
            OP("dve", "tensor_copy", ["rp0", "posi"], ["posi"], out=posi[R], in_=rp[R, 0, :])
            OP("dve", "tensor_copy", ["posi"], ["rp5"], out=rp[R, 5, :], in_=posi[R])
            OP("dve", "tensor_tensor", ["rp0", "rp5"], ["rp0"], out=rp[R, 0, :], in0=rp[R, 0, :], in1=rp[R, 5, :],
               op=ALU.subtract)
            ACT(rp[R, 1, :], rp[R, 0, :], AF.Sin, ["rp0"], ["rp1"], scale=float(np.pi))
            ACT(rp[R, 2, :], rp[R, 0, :], AF.Sin, ["rp0"], ["rp2"], scale=float(np.pi / 2.0))
            OP("dve", "tensor_tensor", ["rp1"], ["rp3"], out=rp[R, 3, :], in0=rp[R, 1, :], in1=rp[R, 1, :], op=ALU.mult)
            OP("dve", "tensor_scalar", ["rp3"], ["rp3"], out=rp[R, 3, :], in0=rp[R, 3, :], scalar1=-2.0, scalar2=1.0,
               op0=ALU.mult, op1=ALU.add)
            OP("dve", "tensor_tensor", ["rp2", "rp5"], ["rp5"], out=rp[R, 5, :], in0=rp[R, 2, :], in1=rp[R, 2, :], op=ALU.mult)
            OP("dve", "tensor_scalar", ["rp5"], ["rp5"], out=rp[R, 5, :], in0=rp[R, 5, :], scalar1=-4.0, scalar2=2.0,
               op0=ALU.mult, op1=ALU.add)
            OP("dve", "tensor_tensor", ["rp5", "rp1"], ["rp4"], out=rp[R, 4, :], in0=rp[R, 5, :], in1=rp[R, 1, :],
               op=ALU.mult)
            OP("dve", "tensor_scalar", ["rp4"], ["rp4"], out=rp[R, 4, :], in0=rp[R, 4, :], scalar1=ropec[R, 1:2],
               scalar2=None, op0=ALU.mult)
            cosR, sinR = rp[R, 3, :], rp[R, 4, :]

            def rope_evac(psA, psB, dst, keyA, keyB, wkey):
                OP("dve", "tensor_tensor", [keyA, "rp3"], ["rt0"], out=rt[R, 0, :], in0=psA, in1=cosR, op=ALU.mult)
                OP("dve", "tensor_tensor", [keyB, "rp4"], ["rt1"], out=rt[R, 1, :], in0=psB, in1=sinR, op=ALU.mult)
                OP("pool", "tensor_tensor", ["rt0", "rt1"], wkey, out=dst, in0=rt[R, 0, :], in1=rt[R, 1, :], op=ALU.add)

            P.mark('kpe g%d' % g)
            for k in range(8):
                MM(PS(0, parts=96), w_a[:, k, 320:416], xT[:, k, :], k == 0, k == 7, ["w_a", "xT"], ["ps0"])
            for k in range(8):
                MM(PS(1, parts=96), w_a[:, k, 416:512], xT[:, k, :], k == 0, k == 7, ["w_a", "xT"], ["ps1"])
            rope_evac(PS(0)[R], PS(1)[R], kpe[R], "ps0", "ps1", ["kpe"])
            for h in range(8):
                OP("pool", "tensor_copy", ["kpe"], ["KT%d" % h], out=KT[R, h, gs], in_=kpe[R])
            P.mark('knope g%d' % g)
            for h in range(8):
                pb = 2 + h % 2
                MM(PS(pb, parts=64), w_ukv[:, h * 64:(h + 1) * 64], latn[:, 2, :], True, True, ["w_ukv", "latn2"], ["ps%d" % pb])
                ACT(KT[0:64, h, gs], PS(pb, parts=64), AF.Copy, ["ps%d" % pb], ["KT%d" % h])
            P.mark('V g%d' % g)
            for t in range(4):
                kt = g * 4 + t
                pb = t % 2
                MM(PS(pb), latn[:, 2, t * 128:(t + 1) * 128], w_ukv[:, 512:1024], True, True, ["latn2", "w_ukv"], ["ps%d" % pb])
                psv = PS(pb).rearrange("p (a e c) -> p a e c", a=4, e=2)
                vav = VA[:, kt].rearrange("p (a e) c -> p a e c", e=2)
                ACT(vav[:, :, 0, 0:64], psv[:, :, 0, :], AF.Copy, ["ps%d" % pb], ["VA%d" % kt])
                OP("dve", "tensor_copy", ["ps%d" % pb], ["VA%d" % kt], out=vav[:, :, 1, 64:128], in_=psv[:, :, 1, :])
            P.mark('q g%d' % g)
            for h in range(8):
                for kk in range(2):
                    MM(PS(2, parts=96), w_uq[:, kk, h * 96:(h + 1) * 96], latn[:, kk, :], kk == 0, kk == 1,
                       ["w_uq", "latn%d" % kk], ["ps2"])
                for kk in range(2):
                    MM(PS(3, parts=96), w_uq[:, kk, 768 + h * 96:768 + (h + 1) * 96], latn[:, kk, :], kk == 0, kk == 1,
                       ["w_uq", "latn%d" % kk], ["ps3"])
                ACT(qT[0:64, h, :], PS(2, parts=64), AF.Copy, ["ps2"], ["qT%d" % h])
                rope_evac(PS(2)[R], PS(3)[R], qT[R, h, :], "ps2", "ps3", ["qT%d" % h])
            P.mark('attn g%d' % g)
            nck = 4 * g + 4
            for h in range(8):
                ob = 4 + h % 2
                pr, hi = h // 2, h % 2

                def s_mm(c):
                    MM(PS(c % 2), KT[0:96, h, c * 128:(c + 1) * 128], qT[0:96, h, :], True, True,
                       ["KT%d" % h, "qT%d" % h], ["ps%d" % (c % 2)])
                s_mm(0)
                for c in range(nck):
                    if c + 1 < nck:
                        s_mm(c + 1)
                    pt = c % 3
                    ACT(PT[:, pt, :], PS(c % 2), AF.Exp, ["ps%d" % (c % 2)], ["PT%d" % pt], scale=SC)
                    if c >= 4 * g:
                        OP("pool", "tensor_tensor", ["PT%d" % pt], ["PT%d" % pt], out=PT[:, pt, :], in0=PT[:, pt, :],
                           in1=masks[:, c - 4 * g, :], op=ALU.mult)
                    MM(PS(ob), VA[:, c, h, :], PT[:, pt, :], c == 0, c == nck - 1, ["VA%d" % c, "PT%d" % pt], ["ps%d" % ob])
                lo, up = (slice(0, 64), slice(64, 128)) if hi == 0 else (slice(64, 128), slice(0, 64))
                ACT(dsb[up, 0, :], PS(ob)[up], AF.Copy, ["ps%d" % ob], ["dsb0"])
                OP("dve", "reciprocal", ["dsb0"], ["dsb1"], out=dsb[lo, 1, :], in_=dsb[up, 0, :])
                OP("dve", "tensor_tensor", ["ps%d" % ob, "dsb1"], ["attnT"], out=attnT[lo, pr, gs], in0=PS(ob)[lo],
                   in1=dsb[lo, 1, :], op=ALU.mult)
        P.barrier_all()
        if stage == "1a":
            DMA("sp", dbg_d[:, 0:4 * S].rearrange("p (k c) -> p k c", k=4), attnT, [], ["dbg"])
            P.emit(final_tiles=["dbg"])
            return nc

        A.release(p1_mark)
        w_b = A.alloc([8, 1544], BF16)
        w_mqk = A.alloc([1024], BF16)
        for k in range(8):
            DMA("pool", w_b[:, k, :], wb_d[k * 128:(k + 1) * 128, :], [], ["w_b"])
        DMA("pool", w_mqk, wmqk_d, [], ["w_mqk"])
        w_c = U[:, 0:16384].rearrange("p (k c) -> p k c", k=8)
        w_brm = U[:, 16384:20480].rearrange("p (k c) -> p k c", k=4)
        w_bra = U[:, 20480:24576].rearrange("p (k c) -> p k c", k=4)
        w_mix = U[:, 24576:32768].rearrange("p (k c) -> p k c", k=8)
        for k in range(8):
            DMA("pool", w_c[:, k, :], wc_d[k * 128:(k + 1) * 128, :], [], ["w_c"])
            DMA("pool", w_mix[:, k, :], wmix_d[k * 128:(k + 1) * 128, :], [], ["w_mix"])
        for k in range(4):
            DMA("pool", w_brm[:, k, :], wbrm_d[k * 128:(k + 1) * 128, :], [], ["w_brm"])
            DMA("pool", w_bra[:, k, :], wbra_d[k * 128:(k + 1) * 128, :], [], ["w_bra"])
        xT = A.alloc([8, 512], BF16)
        xme = A.alloc([4, 520], F32)
        acc = A.alloc([512], F32)
        xcf = A.alloc([4, 512], F32)
        xcb = A.alloc([4, 512], BF16)
        so = A.alloc([4, 512], BF16)
        mq = A.alloc([4, 512], BF16)
        mk = A.alloc([4, 512], BF16)
        vam = A.alloc([4, 130], BF16)
        gt = A.alloc([16], F32)
        gtmp = A.alloc([8], F32)
        lfrep = A.alloc([128], F32)
        DTt = A.alloc([128], F32)
        EB = A.alloc([128], F32)
        DTm = A.alloc([128], F32)
        STb = A.alloc([128], BF16)
        qsb = A.alloc([128], BF16)
        kwb = A.alloc([128], BF16)
        Cf = A.alloc([4, 130], F32)
        Cb = A.alloc([4, 130], BF16)
        sml = A.alloc([16], F32)
        hv = A.alloc([128], F32)
        hnb = A.alloc([128], BF16)
        t1 = A.alloc([128], F32)
        t2 = A.alloc([128], F32)
        sml2 = A.alloc([2, 16], F32)
        hv2 = A.alloc([2, 128], F32)
        hnb2 = A.alloc([2, 128], BF16)
        t12 = A.alloc([2, 128], F32)
        t22 = A.alloc([2, 128], F32)

        def mlstm_second(h, ts_, ss):
            q2 = h % 2
            hb = (5, 1)[q2]
            sm = sml2[:, q2, :]
            k_ = "m2_%d" % q2
            hv_, hnb_, t1_, t2_ = hv2[:, q2, :], hnb2[:, q2, :], t12[:, q2, :], t22[:, q2, :]
            OP("dve", "tensor_copy", ["ps%d" % hb], [k_], out=sm[:, 0:1], in_=PS(hb, 1, c0=128))
            OP("dve", "scalar_tensor_tensor", [k_], [k_], out=sm[:, 1:2], in0=sm[:, 0:1], scalar=-1.0,
               in1=sm[:, 0:1], op0=ALU.mult, op1=ALU.max)
            OP("dve", "tensor_scalar", [k_], [k_], out=sm[:, 2:3], in0=sm[:, 1:2], scalar1=1.0, scalar2=None, op0=ALU.max)
            OP("dve", "reciprocal", [k_], [k_], out=sm[:, 3:4], in_=sm[:, 2:3])
            ACT(hv_, PS(hb, 128), AF.Copy, ["ps%d" % hb, k_], [k_ + "hv"], scale=sm[:, 3:4])
            OP("dve", "bn_stats", [k_ + "hv"], [k_], out=sm[:, 4:10], in_=hv_)
            OP("dve", "bn_aggr", [k_], [k_], out=sm[:, 10:12], in_=sm[:, 4:10])
            ACT(sm[:, 13:14], sm[:, 11:12], AF.Ln, [k_], [k_], bias=eps_c)
            ACT(sm[:, 12:13], sm[:, 13:14], AF.Exp, [k_], [k_], scale=-0.5)
            OP("dve", "tensor_scalar", [k_ + "hv", k_], [k_ + "hn"], out=hnb_, in0=hv_, scalar1=sm[:, 10:11],
               scalar2=sm[:, 12:13], op0=ALU.subtract, op1=ALU.mult)
            OP("pe", "transpose", [k_ + "hn"], ["ps7"], out=PSB(7, 128), in_=hnb_, identity=ident_b)
            OP("dve", "tensor_scalar", ["ps7", "pm"], [k_ + "t1"], out=t1_, in0=PSB(7, 128), scalar1=pm[:, 20 + h:21 + h],
               scalar2=None, op0=ALU.mult)
            OP("dve", "scalar_tensor_tensor", ["xcf%d" % h, "pm", k_ + "t1"], [k_ + "t2"], out=t2_, in0=xcf[:, h, ts_],
               scalar=pm[:, 24 + h:25 + h], in1=t1_, op0=ALU.mult, op1=ALU.add)
            OP("pool", "tensor_tensor", [k_ + "t2", "so%d" % h], ["ymT"], out=ymT[:, h, ss], in0=t2_, in1=so[:, h, ts_],
               op=ALU.mult)

        pend_m = None
        OP("pool", "memset", [], ["Cf%d" % h for h in range(4)], Cf, 0.0)
        OP("pool", "memset", [], ["Cb%d" % h for h in range(4)], Cb, 0.0)
        OP("pool", "memset", [], ["xme%d" % c for c in range(4)], xme, 0.0)
        OP("pool", "memset", [], ["vam"], vam, 1.0)
        DSC = float(128.0 ** -0.5)
        for g in range(NG):
            G = b * NG + g
            gs = slice(g * 512, (g + 1) * 512)
            DMA("sp", xT, xT_d[G].rearrange("p (k c) -> p k c", k=8), ["xT_d%d" % G], ["xT"])
            for c in range(4):
                pb = c % 2
                for k in range(8):
                    MM(PS(pb), w_b[:, k, c * 128:(c + 1) * 128], xT[:, k, :], k == 0, k == 7, ["w_b", "xT"], ["ps%d" % pb])
                ACT(xme[:, c, 3:515], PS(pb), AF.Copy, ["ps%d" % pb], ["xme%d" % c])
                OP("dve", "tensor_scalar", ["xme%d" % c, "pm"], ["acc"], out=acc, in0=xme[:, c, 0:512],
                   scalar1=pm[:, c * 4:c * 4 + 1], scalar2=None, op0=ALU.mult)
                for j in range(1, 4):
                    OP("dve", "scalar_tensor_tensor", ["xme%d" % c, "pm", "acc"], ["acc"], out=acc, in0=xme[:, c, j:j + 512],
                       scalar=pm[:, c * 4 + j:c * 4 + j + 1], in1=acc, op0=ALU.mult, op1=ALU.add)
                ACT(xcf[:, c, :], acc, AF.Silu, ["acc", "pm"], ["xcf%d" % c], bias=pm[:, 16 + c:17 + c])
                OP("pool", "tensor_copy", ["xcf%d" % c], ["xcb%d" % c], out=xcb[:, c, :], in_=xcf[:, c, :])
                OP("pool", "tensor_copy", ["xme%d" % c], ["xme%d" % c], out=xme[:, c, 0:3], in_=xme[:, c, 512:515])
            for c in range(4):
                pb = c % 2
                for k in range(8):
                    MM(PS(pb), w_b[:, k, 1024 + c * 128:1024 + (c + 1) * 128], xT[:, k, :], k == 0, k == 7,
                       ["w_b", "xT"], ["ps%d" % pb])
                ACT(so[:, c, :], PS(pb), AF.Sigmoid, ["ps%d" % pb], ["so%d" % c])
            for h in range(4):
                MM(PS(0), w_mqk[:, h * 128:(h + 1) * 128], xcb[:, h, :], True, True, ["w_mqk", "xcb%d" % h], ["ps0"])
                ACT(mq[:, h, :], PS(0), AF.Copy, ["ps0"], ["mq%d" % h])
                MM(PS(1), w_mqk[:, 512 + h * 128:512 + (h + 1) * 128], xcb[:, h, :], True, True, ["w_mqk", "xcb%d" % h], ["ps1"])
                ACT(mk[:, h, :], PS(1), AF.Copy, ["ps1"], ["mk%d" % h], scale=DSC)
            for t in range(4):
                ts_ = slice(t * 128, (t + 1) * 128)
                ss = slice(g * 512 + t * 128, g * 512 + (t + 1) * 128)
                for k in range(8):
                    MM(PS(0), xT[:, k, ts_], w_b[:, k, 512:1024], k == 0, k == 7, ["xT", "w_b"], ["ps0"])
                for k in range(8):
                    MM(PS(2, 8, c0=256), xT[:, k, ts_], w_b[:, k, 1536:1544], k == 0, k == 7, ["xT", "w_b"], ["ps2c"])
                ACT(vam[:, :, 0:128], PS(0).rearrange("p (h c) -> p h c", h=4), AF.Copy, ["ps0"], ["vam"])
                OP("dve", "tensor_tensor", ["ps2c", "gbt"], ["gt"], out=gt[:, 0:8], in0=PS(2, 8, c0=256), in1=gbt, op=ALU.add)
                ACT(gtmp[:, 0:4], gt[:, 4:8], AF.Exp, ["gt"], ["gtmp"], scale=-1.0)
                ACT(gtmp[:, 4:8], gtmp[:, 0:4], AF.Ln, ["gtmp"], ["gtmp2"], bias=one_c)
                OP("dve", "tensor_scalar", ["gtmp2"], ["lf"], out=gt[:, 8:12], in0=gtmp[:, 4:8], scalar1=-1.0, scalar2=None,
                   op0=ALU.mult)
                MM(PS(2, 4, c0=128), tri_f, gt[:, 8:12], True, True, ["lf"], ["ps2b"])
                OP("dve", "tensor_tensor", ["gt", "ps2b"], ["ccol"], out=gt[:, 12:16], in0=gt[:, 0:4], in1=PS(2, 4, c0=128),
                   op=ALU.subtract)
                for h in range(4):
                    OP("dve", "tensor_scalar", ["lf"], ["lfrep"], out=lfrep, in0=ones_f, scalar1=gt[:, 8 + h:9 + h],
                       scalar2=None, op0=ALU.mult)
                    MM(PS(2, 128), lfrep, tri_f, True, True, ["lfrep"], ["ps2a"])
                    ACT(DTt, PS(2, 128), AF.Exp, ["ps2a", "ccol"], ["DT"], bias=gt[:, 12 + h:13 + h])
                    ACT(EB, PS(2, 128), AF.Exp, ["ps2a"], ["EB"])
                    OP("pool", "tensor_tensor", ["DT"], ["DTm"], out=DTm, in0=DTt, in1=tri_f, op=ALU.mult)
                    MM(PS(3, 128), mk[:, h, ts_], mq[:, h, ts_], True, True, ["mk%d" % h, "mq%d" % h], ["ps3"])
                    OP("dve", "tensor_tensor", ["ps3", "DTm"], ["STb"], out=STb, in0=PS(3, 128), in1=DTm, op=ALU.mult)
                    OP("pool", "tensor_tensor", ["mq%d" % h, "EB"], ["qsb"], out=qsb, in0=mq[:, h, ts_], in1=EB, op=ALU.mult)
                    MM(PS(4, 128), xcb[:, h, ts_], w_mqk[:, 512 + h * 128:512 + (h + 1) * 128], True, True,
                       ["xcb%d" % h, "w_mqk"], ["ps4"])
                    OP("dve", "tensor_scalar", ["ps4", "DT"], ["kwb"], out=kwb, in0=PS(4, 128), scalar1=DTt[:, 127:128],
                       scalar2=DSC, op0=ALU.mult, op1=ALU.mult)
                    hb = (5, 1)[h % 2]
                    MM(PS(hb, 129), STb, vam[:, h, 0:129], True, False, ["STb", "vam"], ["ps%d" % hb])
                    MM(PS(hb, 129), qsb, Cb[:, h, 0:129], False, True, ["qsb", "Cb%d" % h], ["ps%d" % hb])
                    MM(PS(6, 129), kwb, vam[:, h, 0:129], True, True, ["kwb", "vam"], ["ps6"])
                    OP("dve", "scalar_tensor_tensor", ["Cf%d" % h, "EB", "ps6"], ["Cf%d" % h], out=Cf[:, h, 0:129],
                       in0=Cf[:, h, 0:129], scalar=EB[:, 127:128], in1=PS(6, 129), op0=ALU.mult, op1=ALU.add)
                    ACT(Cb[:, h, 0:129], Cf[:, h, 0:129], AF.Copy, ["Cf%d" % h], ["Cb%d" % h])
                    if pend_m is not None:
                        mlstm_second(*pend_m)
                    pend_m = (h, ts_, ss)
            if pend_m is not None:
                mlstm_second(*pend_m)
                pend_m = None
        P.barrier_all()
        if stage == "1b":
            DMA("sp", dbg_d[:, 0:4 * S].rearrange("p (k c) -> p k c", k=4), attnT, [], ["dbg"])
            DMA("sp", dbg_d[:, 4 * S:8 * S].rearrange("p (k c) -> p k c", k=4), ymT, [], ["dbg"])
            P.emit(final_tiles=["dbg"])
            return nc

        A.release(p1_mark)
        xT = A.alloc([8, 512], BF16)
        sg = A.alloc([16, 512], BF16)
        ma = A.alloc([2, 512], F32)
        mixin = A.alloc([8, 512], BF16)
        lng = A.alloc([1024], F32)
        lnb = A.alloc([1024], F32)
        xf = A.alloc([2, 1024], F32)
        pre = A.alloc([2, 1024], F32)
        h1t = A.alloc([2, 1024], F32)
        lsm = A.alloc([2, 16], F32)
        DMA("sp", lng, lnp_d[0:1, :].broadcast_to([128, 1024]), [], ["lnp"])
        DMA("sp", lnb, lnp_d[1:2, :].broadcast_to([128, 1024]), [], ["lnp"])
        for g in range(NG):
            G = b * NG + g
            gs = slice(g * 512, (g + 1) * 512)
            DMA("sp", xT, xT_d[G].rearrange("p (k c) -> p k c", k=8), ["xT_d%d" % G], ["xT"])
            for c in range(16):
                pb = c % 2
                for k in range(8):
                    MM(PS(pb), w_c[:, k, c * 128:(c + 1) * 128], xT[:, k, :], k == 0, k == 7, ["w_c", "xT"], ["ps%d" % pb])
                ACT(sg[:, c, :], PS(pb), AF.Sigmoid, ["ps%d" % pb], ["sg%d" % c])
            for c in range(8):
                for kk in range(4):
                    MM(PS(2), w_brm[:, kk, c * 128:(c + 1) * 128], ymT[:, kk, gs], kk == 0, kk == 3, ["w_brm", "ymT"], ["ps2"])
                for kk in range(4):
                    MM(PS(3), w_bra[:, kk, c * 128:(c + 1) * 128], attnT[:, kk, gs], kk == 0, kk == 3, ["w_bra", "attnT"], ["ps3"])
                OP("dve", "tensor_tensor", ["ps2", "sg%d" % c], ["ma0"], out=ma[:, 0, :], in0=PS(2), in1=sg[:, c, :], op=ALU.mult)
                OP("dve", "tensor_tensor", ["ps3", "sg%d" % (8 + c)], ["ma1"], out=ma[:, 1, :], in0=PS(3), in1=sg[:, 8 + c, :],
                   op=ALU.mult)
                OP("pool", "tensor_tensor", ["ma0", "ma1"], ["mixin"], out=mixin[:, c, :], in0=ma[:, 0, :], in1=ma[:, 1, :],
                   op=ALU.add)
            for t in range(4):
                tt = G * 4 + t
                s2 = t % 2
                DMA("sp", xf[:, s2, :], x_d[tt * 128:(tt + 1) * 128, :], [], ["xf%d" % s2])
                for half in range(2):
                    for k in range(8):
                        MM(PS(4 + half), mixin[:, k, t * 128:(t + 1) * 128], w_mix[:, k, half * 512:(half + 1) * 512],
                           k == 0, k == 7, ["mixin", "w_mix"], ["ps%d" % (4 + half)])
                layer_norm_tile(xf[:, s2, :], PS(4), PS(5), lng, lnb, pre[:, s2, :], h1t[:, s2, :], lsm[:, s2, 0:12],
                                lsm[:, s2, 12:15], lsm[:, s2, 15:16], "ln%d" % s2, ["xf%d" % s2], ["ps4", "ps5"])
                DMA("sp", h1_d[tt * 128:(tt + 1) * 128, :], h1t[:, s2, :], ["ln%dout" % s2], ["h1_d%d" % s2])
        P.barrier_all()
        if stage == "1c":
            P.emit(final_tiles=[])
            return nc

    if stage == "1":
        P.emit(final_tiles=[])
        return nc
    A.release(base_mark)
    g4_all = A.alloc([T // 128, 4], F32)
    slot_all = A.alloc([T // 128, 4], I32)
    keep_mark = A.mark()
    w_cq = A.alloc([8, 1024], BF16)
    w_co = A.alloc([8, 1024], BF16)
    w_ckv = A.alloc([8, 2048], BF16)
    w_rt = A.alloc([8, 32], F32)
    brt = A.alloc([32], F32)
    lng = A.alloc([1024], F32)
    lnb = A.alloc([1024], F32)
    for k in range(8):
        DMA("pool", w_cq[:, k, :], wcq_d[k * 128:(k + 1) * 128, :], [], ["w_cq"])
        DMA("pool", w_co[:, k, :], wco_d[k * 128:(k + 1) * 128, :], [], ["w_co"])
        DMA("pool", w_ckv[:, k, :], wckv_d[k * 128:(k + 1) * 128, :], [], ["w_ckv"])
    DMA("sp", w_rt, wr_d.rearrange("(k p) e -> p k e", p=128), [], ["w_rt"])
    DMA("sp", brt, br_d[0:1, :].broadcast_to([128, NE]), [], ["brt"])
    DMA("sp", lng, lnp_d[2:3, :].broadcast_to([128, 1024]), [], ["lnp"])
    DMA("sp", lnb, lnp_d[3:4, :].broadcast_to([128, 1024]), [], ["lnp"])
    memf = A.alloc([1024], F32)
    memb = A.alloc([1024], BF16)
    memT = A.alloc([8, 256], BF16)
    KmT = A.alloc([8, 256], BF16)
    Vm = A.alloc([2, 1024], BF16)
    h1f = A.alloc([4, 1024], F32)
    h1b = A.alloc([1024], BF16)
    h1T = A.alloc([8, 512], BF16)
    qcT = A.alloc([8, 512], BF16)
    PT2 = A.alloc([2, 512], BF16)
    rden = A.alloc([512], F32)
    oT = A.alloc([8, 512], BF16)
    pre = A.alloc([2, 1024], F32)
    h2t = A.alloc([2, 1024], F32)
    h2b = A.alloc([2, 1024], BF16)
    lsm = A.alloc([2, 16], F32)
    h2T = A.alloc([8, 128], F32)
    lg = A.alloc([32], F32)
    m8 = A.alloc([8], F32)
    rsm = A.alloc([16], F32)
    maskb = A.alloc([32], BF16)
    if skip1:
        zt = A.alloc([4, 1024], BF16)
        OP("pool", "memset", [], ["zt"], zt, 0.0)
        xs_v = xs_d.rearrange("(n j p) d -> n p j d", p=128, j=4)
        for n in range(NSLOT // 512):
            DMA("sp", xs_v[n], zt, ["zt"], ["xs_zero"])
    carry = A.alloc([32], F32)
    lim = A.alloc([32], F32)
    slotm = A.alloc([32], F32)
    ovf = A.alloc([32], F32)
    oh = A.alloc([32], F32)
    s4f = A.alloc([4], F32)
    OP("pool", "iota", [], ["carry"], carry, [[CAP, NE]], base=0, channel_multiplier=0, allow_small_or_imprecise_dtypes=True)
    OP("pool", "iota", [], ["lim"], lim, [[CAP, NE]], base=CAP, channel_multiplier=0, allow_small_or_imprecise_dtypes=True)
    def router_tile(tt, s2):
        P.mark('p2 router %d' % tt)
        for k in range(8):
            MM(PS(4 + k // 4, 128, c0=(k % 4) * 128), h2t[:, s2, k * 128:(k + 1) * 128], ident_f, True, True,
               ["l2%dout" % s2], ["ps%d" % (4 + k // 4)])
        for hh in range(2):
            OP("dve", "tensor_copy", ["ps%d" % (4 + hh)], ["h2T"], out=h2T[:, hh * 4:(hh + 1) * 4, :],
               in_=PS(4 + hh).rearrange("p (k c) -> p k c", k=4))
        for k in range(8):
            MM(PS(2, 32), h2T[:, k, :], w_rt[:, k, :], k == 0, k == 7, ["h2T", "w_rt"], ["ps2"])
        OP("dve", "tensor_tensor", ["ps2", "brt"], ["lg"], out=lg, in0=PS(2, 32), in1=brt, op=ALU.add)
        OP("dve", "max", ["lg"], ["m8"], out=m8, in_=lg)
        OP("dve", "tensor_scalar", ["m8"], ["rs0"], out=rsm[:, 0:1], in0=m8[:, 0:1], scalar1=-1.0, scalar2=None,
           op0=ALU.mult)
        ACT(rsm[:, 4:8], m8[:, 0:4], AF.Exp, ["m8", "rs0"], ["rs4"], bias=rsm[:, 0:1], accum_out=rsm[:, 1:2])
        OP("dve", "reciprocal", ["rs4"], ["rs2"], out=rsm[:, 2:3], in_=rsm[:, 1:2])
        OP("dve", "tensor_scalar", ["rs4", "rs2"], ["g4"], out=g4_all[:, tt, :], in0=rsm[:, 4:8], scalar1=rsm[:, 2:3],
           scalar2=None, op0=ALU.mult)
        OP("dve", "tensor_scalar", ["lg", "m8"], ["maskb"], out=maskb, in0=lg, scalar1=m8[:, 3:4], scalar2=None,
           op0=ALU.is_ge)
        MM(PS(3, 32), stri_b, maskb, True, True, ["maskb"], ["ps3"])
        MM(PS(3, 32, c0=64), ones_b, maskb, True, True, ["maskb"], ["ps3"])
        OP("dve", "tensor_tensor", ["ps3", "carry"], ["slotm"], out=slotm, in0=PS(3, 32), in1=carry, op=ALU.add)
        OP("dve", "tensor_tensor", ["ps3", "carry"], ["carry"], out=carry, in0=PS(3, 32, c0=64), in1=carry, op=ALU.add)
        OP("dve", "tensor_tensor", ["slotm", "lim"], ["ovf"], out=ovf, in0=slotm, in1=lim, op=ALU.is_ge)
        OP("dve", "scalar_tensor_tensor", ["ovf", "slotm"], ["slotm"], out=slotm, in0=ovf, scalar=BIG, in1=slotm,
           op0=ALU.mult, op1=ALU.add)
        for k4 in range(4):
            OP("dve", "tensor_scalar", ["lg", "m8"], ["oh"], out=oh, in0=lg, scalar1=m8[:, k4:k4 + 1], scalar2=None,
               op0=ALU.is_equal)
            OP("dve", "tensor_tensor", ["oh", "slotm"], ["oh"], out=oh, in0=oh, in1=slotm, op=ALU.mult)
            OP("dve", "reduce_sum", ["oh"], ["s4f"], out=s4f[:, k4:k4 + 1], in_=oh, axis=mybir.AxisListType.X)
        OP("dve", "tensor_copy", ["s4f"], ["slot"], out=slot_all[:, tt, :], in_=s4f)
        OP("dve", "tensor_single_scalar", ["s4f"], ["s4f"], out=s4f, in_=s4f, scalar=float(NSLOT), op=ALU.is_lt)
        OP("dve", "tensor_tensor", ["s4f", "g4"], ["g4"], out=g4_all[:, tt, :], in0=g4_all[:, tt, :], in1=s4f, op=ALU.mult)
        P.mark('p2 scatter %d' % tt)
        for k4 in range(4):
            P.op("pool", (lambda e, tt=tt, k4=k4, s2=s2: e.indirect_dma_start(
                out=xs_d, out_offset=bass.IndirectOffsetOnAxis(ap=slot_all[:, tt, k4:k4 + 1], axis=0),
                in_=h2b[:, s2, :], in_offset=None, bounds_check=bc_reg(e), oob_is_err=False)),
                reads=["h2b%d" % s2, "slot", "xs_zero"], writes=["xs_w%d" % s2], dma=True)

    pend_r = None
    P.mark('p2 start')
    for b in range(NB):
        P.mark('p2 mem b%d' % b)
        for mt in range(2):
            DMA("sp", memf, mem_d[b * 256 + mt * 128:b * 256 + (mt + 1) * 128, :], [], ["memf"])
            ACT(memb, memf, AF.Copy, ["memf"], ["memb"])
            for k in range(8):
                OP("pe", "transpose", ["memb"], ["ps6"], out=PSB(6, 128, c0=k * 128), in_=memb[:, k * 128:(k + 1) * 128],
                   identity=ident_b)
            OP("dve", "tensor_copy", ["ps6"], ["memT"], out=memT[:, :, mt * 128:(mt + 1) * 128],
               in_=PSB(6).rearrange("p (k c) -> p k c", k=8))
        for c in range(8):
            pb = c % 2
            for k in range(8):
                MM(PS(pb, 256), w_ckv[:, k, c * 128:(c + 1) * 128], memT[:, k, :], k == 0, k == 7, ["w_ckv", "memT"], ["ps%d" % pb])
            ACT(KmT[:, c, :], PS(pb, 256), AF.Copy, ["ps%d" % pb], ["KmT"])
        for mt in range(2):
            for half in range(2):
                pb = half
                for k in range(8):
                    MM(PS(pb), memT[:, k, mt * 128:(mt + 1) * 128], w_ckv[:, k, 1024 + half * 512:1024 + (half + 1) * 512],
                       k == 0, k == 7, ["memT", "w_ckv"], ["ps%d" % pb])
                ACT(Vm[:, mt, half * 512:(half + 1) * 512], PS(pb), AF.Copy, ["ps%d" % pb], ["Vm"])
        for g in range(NG):
            G = b * NG + g
            P.mark('p2 grp %d' % G)
            for t in range(4):
                tt = G * 4 + t
                DMA("sp", h1f[:, t, :], h1_d[tt * 128:(tt + 1) * 128, :], [], ["h1f%d" % t])
                ACT(h1b, h1f[:, t, :], AF.Copy, ["h1f%d" % t], ["h1b"])
                s2 = t % 2
                for k in range(8):
                    OP("pe", "transpose", ["h1b"], ["ps%d" % (6 + s2)], out=PSB(6 + s2, 128, c0=k * 128),
                       in_=h1b[:, k * 128:(k + 1) * 128], identity=ident_b)
                OP("dve", "tensor_copy", ["ps%d" % (6 + s2)], ["h1T"], out=h1T[:, :, t * 128:(t + 1) * 128],
                   in_=PSB(6 + s2).rearrange("p (k c) -> p k c", k=8))
            for c in range(8):
                pb = c % 2
                for k in range(8):
                    MM(PS(pb), w_cq[:, k, c * 128:(c + 1) * 128], h1T[:, k, :], k == 0, k == 7, ["w_cq", "h1T"], ["ps%d" % pb])
                ACT(qcT[:, c, :], PS(pb), AF.Copy, ["ps%d" % pb], ["qcT"])
            P.mark('p2 attn %d' % G)
            for h in range(4):
                for mt in range(2):
                    for kk in range(2):
                        MM(PS(mt), KmT[:, 2 * h + kk, mt * 128:(mt + 1) * 128], qcT[:, 2 * h + kk, :], kk == 0, kk == 1,
                           ["KmT", "qcT"], ["ps%d" % mt])
                    ACT(PT2[:, mt, :], PS(mt), AF.Exp, ["ps%d" % mt], ["PT2%d" % mt], scale=1.0 / 16.0)
                for mt in range(2):
                    MM(PS(2), ones_b, PT2[:, mt, :], mt == 0, mt == 1, ["PT2%d" % mt], ["ps2"])
                OP("dve", "reciprocal", ["ps2"], ["rden"], out=rden, in_=PS(2))
                for cc in range(2):
                    for mt in range(2):
                        MM(PS(3 + cc), Vm[:, mt, h * 256 + cc * 128:h * 256 + (cc + 1) * 128], PT2[:, mt, :], mt == 0, mt == 1,
                           ["Vm", "PT20", "PT21"], ["ps%d" % (3 + cc)])
                    OP("dve", "tensor_tensor", ["ps%d" % (3 + cc), "rden"], ["oT"], out=oT[:, 2 * h + cc, :], in0=PS(3 + cc),
                       in1=rden, op=ALU.mult)
            for t in range(4):
                tt = G * 4 + t
                s2 = t % 2
                for half in range(2):
                    for k in range(8):
                        MM(PS(half), oT[:, k, t * 128:(t + 1) * 128], w_co[:, k, half * 512:(half + 1) * 512], k == 0, k == 7,
                           ["oT", "w_co"], ["ps%d" % half])
                layer_norm_tile(h1f[:, t, :], PS(0), PS(1), lng, lnb, pre[:, s2, :], h2t[:, s2, :], lsm[:, s2, 0:12],
                                lsm[:, s2, 12:15], lsm[:, s2, 15:16], "l2%d" % s2, ["h1f%d" % t], ["ps0", "ps1"])
                DMA("sp", h2_d[tt * 128:(tt + 1) * 128, :], h2t[:, s2, :], ["l2%dout" % s2], ["h2_d%d" % s2])
                ACT(h2b[:, s2, :], h2t[:, s2, :], AF.Copy, ["l2%dout" % s2], ["h2b%d" % s2])
                if pend_r is not None:
                    router_tile(*pend_r)
                pend_r = (tt, s2)
            if pend_r is not None:
                router_tile(*pend_r)
                pend_r = None
    P.barrier_all()
    if stage == "2":
        DMA("sp", dbg2_d[:, 0:128], g4_all.rearrange("p a b -> p (a b)"), [], ["dbg2"])
        DMA("sp", dbg2_d[:, 128:256].bitcast(I32), slot_all.rearrange("p a b -> p (a b)"), [], ["dbg2"])
        P.emit(final_tiles=["dbg2"])
        return nc

    A.release(keep_mark)
    wg = A.alloc([2, 8, 1024], BF16)
    wl = A.alloc([2, 8, 1024], BF16)
    wd = A.alloc([2, 8, 1024], BF16)
    bdn = A.alloc([2, 1024], BF16, parts=1)
    bgl = A.alloc([8, 64], F32)
    braw = A.alloc([2048], F32, parts=32)
    xtok = A.alloc([2, 5, 1024], BF16)
    XT = A.alloc([2, 8, CAP], BF16)
    actT = A.alloc([8, CAP], BF16)
    glu = A.alloc([2, 512], F32)
    sig = A.alloc([2, 512], F32)
    gsx = A.alloc([2, 512], F32)
    lin1 = A.alloc([2, 512], F32)
    yt = A.alloc([2, 1024], BF16)
    DMA("sp", braw[:, 0:1024], bglu_d, [], ["braw"])
    DMA("sp", braw[:, 1024:2048], blin_d, [], ["braw"])
    for c in range(8):
        MM(PS(0, 32, c0=c * 64), braw[:, c * 128:(c + 1) * 128], ident_f[0:32, 0:32], True, True, ["braw"], ["ps0"])
        MM(PS(0, 32, c0=c * 64 + 32), braw[:, 1024 + c * 128:1024 + (c + 1) * 128], ident_f[0:32, 0:32], True, True,
           ["braw"], ["ps0"])
    OP("dve", "tensor_copy", ["ps0"], ["bgl"], out=bgl, in_=PS(0).rearrange("p (c e) -> p c e", c=8))
    OP("dve", "tensor_scalar", ["bgl"], ["bgl"], out=bgl[:, :, 32:64], in0=bgl[:, :, 32:64], scalar1=1.0, scalar2=None,
       op0=ALU.add)
    TG = ((0, 512), (512, CAP - 512))

    def load_expert(e):
        s2 = e % 2
        for k in range(8):
            DMA("pool", wg[:, s2, k, :], wglu_d[e, k * 128:(k + 1) * 128, :], [], ["wg%d" % s2])
            DMA("pool", wl[:, s2, k, :], wlin_d[e, k * 128:(k + 1) * 128, :], [], ["wl%d" % s2])
            DMA("pool", wd[:, s2, k, :], wdn_d[e, k * 128:(k + 1) * 128, :], [], ["wd%d" % s2])
        DMA("pool", bdn[:, s2, :], bdn_d[e:e + 1, :], [], ["bdn%d" % s2])

    def prep_expert(e):
        x2 = e % 2
        DMA("sp", xtok[:, x2], xs_d[e * CAP:(e + 1) * CAP, :].rearrange("(j p) d -> p j d", p=128), [], ["xtok%d" % x2])
        for j in range(5):
            pb = 6 + j % 2
            for k in range(8):
                OP("pe", "transpose", ["xtok%d" % x2], ["ps%d" % pb], out=PSB(pb, 128, c0=k * 128),
                   in_=xtok[:, x2, j, k * 128:(k + 1) * 128], identity=ident_b)
            OP("dve" if j % 2 == 0 else "act", "tensor_copy" if j % 2 == 0 else "copy", ["ps%d" % pb], ["XT%d" % x2],
               out=XT[:, x2, :, j * 128:(j + 1) * 128], in_=PSB(pb).rearrange("p (k c) -> p k c", k=8))

    load_expert(0)
    for e in range(NE):
        s2 = e % 2
        if e + 1 < NE:
            load_expert(e + 1)
        if e == 0:
            prep_expert(0)
        def gu_tail(f, gi, t0, tn):
            OP("pool", "tensor_tensor", ["glu%d" % gi, "sig%d" % gi], ["gsx%d" % gi], out=gsx[:, gi, 0:tn], in0=glu[:, gi, 0:tn],
               in1=sig[:, gi, 0:tn], op=ALU.mult)
            OP("pool", "tensor_scalar", ["lin%d" % gi], ["lin%d" % gi], out=lin1[:, gi, 0:tn], in0=lin1[:, gi, 0:tn],
               scalar1=8.0, scalar2=-6.0, op0=ALU.min, op1=ALU.max)
            OP("dve", "tensor_tensor", ["lin%d" % gi, "gsx%d" % gi], ["actT"], out=actT[:, f, t0:t0 + tn],
               in0=lin1[:, gi, 0:tn], in1=gsx[:, gi, 0:tn], op=ALU.mult)

        pending = None
        for f in range(8):
            for gi, (t0, tn) in enumerate(TG):
                pg, pl = 0 + gi, 2 + gi
                for k in range(8):
                    MM(PS(pl, tn), wl[:, s2, k, f * 128:(f + 1) * 128], XT[:, s2, k, t0:t0 + tn], k == 0, k == 7,
                       ["wl%d" % s2, "XT%d" % s2], ["ps%d" % pl])
                for k in range(8):
                    MM(PS(pg, tn), wg[:, s2, k, f * 128:(f + 1) * 128], XT[:, s2, k, t0:t0 + tn], k == 0, k == 7,
                       ["wg%d" % s2, "XT%d" % s2], ["ps%d" % pg])
                ACT(lin1[:, gi, 0:tn], PS(pl, tn), AF.Identity, ["ps%d" % pl, "bgl"], ["lin%d" % gi], bias=bgl[:, f, 32 + e:33 + e])
                OP("dve", "tensor_scalar", ["ps%d" % pg, "bgl"], ["glu%d" % gi], out=glu[:, gi, 0:tn], in0=PS(pg, tn),
                   scalar1=bgl[:, f, e:e + 1], scalar2=7.0, op0=ALU.add, op1=ALU.min)
                ACT(sig[:, gi, 0:tn], glu[:, gi, 0:tn], AF.Sigmoid, ["glu%d" % gi], ["sig%d" % gi], scale=1.702)
                if pending is not None:
                    gu_tail(*pending)
                pending = (f, gi, t0, tn)
        gu_tail(*pending)
        if e + 1 < NE:
            prep_expert(e + 1)
        for j in range(5):
            y2 = j % 2
            for half in range(2):
                pb = 4 + half
                for k in range(8):
                    MM(PS(pb), actT[:, k, j * 128:(j + 1) * 128], wd[:, s2, k, half * 512:(half + 1) * 512], k == 0, False,
                       ["actT", "wd%d" % s2], ["ps%d" % pb])
                MM(PS(pb), ones_b[0:1, :], bdn[:, s2, half * 512:(half + 1) * 512], False, True, ["bdn%d" % s2], ["ps%d" % pb])
            ACT(yt[:, y2, 0:512], PS(4), AF.Copy, ["ps4"], ["yt%da" % y2])
            OP("dve", "tensor_copy", ["ps5"], ["yt%db" % y2], out=yt[:, y2, 512:1024], in_=PS(5))
            DMA("sp", ys_d[e * CAP + j * 128:e * CAP + (j + 1) * 128, :], yt[:, y2, :], ["yt%da" % y2, "yt%db" % y2],
                ["ys_w%d" % y2])
    P.barrier_all()

    A.release(keep_mark)
    lng = A.alloc([1024], F32)
    lnb = A.alloc([1024], F32)
    NBUF = 3
    yk = A.alloc([NBUF, 4, 1024], BF16)
    macc = A.alloc([2, 1024], F32)
    h2r = A.alloc([NBUF, 1024], F32)
    pre = A.alloc([2, 1024], F32)
    ot = A.alloc([2, 1024], F32)
    lsm = A.alloc([2, 16], F32)
    DMA("sp", lng, lnp_d[4:5, :].broadcast_to([128, 1024]), [], ["lnp"])
    DMA("sp", lnb, lnp_d[5:6, :].broadcast_to([128, 1024]), [], ["lnp"])
    OP("pool", "memset", [], ["yk%d_%d" % (a_, k4) for a_ in range(NBUF) for k4 in range(4)], yk, 0.0)

    def fetch(tt):
        s3 = tt % NBUF
        DMA("sp", h2r[:, s3, :], h2_d[tt * 128:(tt + 1) * 128, :], [], ["h2r%d" % s3])
        for k4 in range(4):
            P.op("pool", (lambda e, tt=tt, k4=k4, s3=s3: e.indirect_dma_start(
                out=yk[:, s3, k4, :], out_offset=None, in_=ys_d,
                in_offset=bass.IndirectOffsetOnAxis(ap=slot_all[:, tt, k4:k4 + 1], axis=0),
                bounds_check=bc_reg(e), oob_is_err=False)), reads=[], writes=["yk%d_%d" % (s3, k4)], dma=True)

    fetch(0)
    fetch(1)
    for tt in range(T // 128):
        s2 = tt % 2
        s3 = tt % NBUF
        if tt + 2 < T // 128:
            fetch(tt + 2)
        OP("dve", "tensor_scalar", ["yk%d_0" % s3], ["macc%d" % s2], out=macc[:, s2, :], in0=yk[:, s3, 0, :],
           scalar1=g4_all[:, tt, 0:1], scalar2=None, op0=ALU.mult)
        for k4 in range(1, 4):
            OP("dve", "scalar_tensor_tensor", ["yk%d_%d" % (s3, k4), "macc%d" % s2], ["macc%d" % s2], out=macc[:, s2, :],
               in0=yk[:, s3, k4, :], scalar=g4_all[:, tt, k4:k4 + 1], in1=macc[:, s2, :], op0=ALU.mult, op1=ALU.add)
        layer_norm_tile(h2r[:, s3, :], macc[:, s2, 0:512], macc[:, s2, 512:1024], lng, lnb, pre[:, s2, :], ot[:, s2, :],
                        lsm[:, s2, 0:12], lsm[:, s2, 12:15], lsm[:, s2, 15:16], "l3%d" % s2, ["h2r%d" % s3],
                        ["macc%d" % s2, "macc%d" % s2])
        DMA("sp", out_d[tt * 128:(tt + 1) * 128, :], ot[:, s2, :], ["l3%dout" % s2], ["out%d" % s2])
    P.emit(final_tiles=["out0", "out1"])
    return nc


def _host_layout(inp):
    f = lambda a: np.ascontiguousarray(np.asarray(a), dtype=np.float32)
    w_in = f(inp["w_in"])[0]
    w_a = np.concatenate([w_in[:, 0:416], w_in[:, 320:384], w_in[:, 400:416], w_in[:, 384:400]], axis=1)
    w_b = w_in[:, 416:1960]
    w_c = w_in[:, 1960:4008]
    wuq = f(inp["w_uq"])[0].reshape(256, 8, 96)
    wuq_sw = np.concatenate([wuq[:, :, 0:64], wuq[:, :, 80:96], wuq[:, :, 64:80]], axis=2)
    w_uq2 = np.concatenate([wuq.reshape(256, 768), wuq_sw.reshape(256, 768)], axis=1)
    wukv = f(inp["w_ukv"])[0].reshape(128, 8, 128)
    w_ukv2 = np.concatenate([wukv[:, :, 0:64].reshape(128, 512), wukv[:, :, 64:128].reshape(128, 512)], axis=1)
    w_mq = f(inp["w_mq"])[0].transpose(1, 0, 2).reshape(128, 512)
    w_mk = f(inp["w_mk"])[0].transpose(1, 0, 2).reshape(128, 512)
    w_mqk = np.concatenate([w_mq, w_mk], axis=1)
    pm = np.zeros((128, 32), np.float32)
    cw = f(inp["conv_w"])[0]
    pm[:, 0:16] = cw.T.reshape(4, 128, 4).transpose(1, 0, 2).reshape(128, 16)
    pm[:, 16:20] = f(inp["conv_b"])[0].reshape(4, 128).T
    pm[:, 20:24] = f(inp["g_mhead"])[0].reshape(4, 128).T
    pm[:, 24:28] = f(inp["w_mskip"])[0].reshape(4, 128).T
    pm[:, 28:30] = f(inp["g_qlat"])[0].reshape(2, 128).T
    pm[:, 30] = f(inp["g_kvlat"])[0]
    gb = np.concatenate([f(inp["b_igate"])[0], f(inp["b_fgate"])[0]])[None, :]
    lnp = np.stack([f(inp[k])[0] for k in ("ln1_g", "ln1_b", "ln2_g", "ln2_b", "ln3_g", "ln3_b")])
    w_gu = f(inp["w_gu"])[0]
    b_gu = f(inp["b_gu"])[0]
    shared = {
        "w_a": w_a, "w_b": w_b, "w_c": w_c, "w_uq2": w_uq2, "w_ukv2": w_ukv2, "w_mqk": w_mqk, "pm": pm, "gb": gb,
        "lnp": lnp, "w_br_m": f(inp["w_br_m"])[0], "w_br_a": f(inp["w_br_a"])[0], "w_mix": f(inp["w_mix_out"])[0],
        "w_cq": f(inp["w_cq"])[0], "w_ckv": f(inp["w_ckv"])[0], "w_co": f(inp["w_co"])[0],
        "w_router": f(inp["w_router"])[0], "b_router": f(inp["b_router"]),
        "w_glu": w_gu[:, :, 0::2], "w_lin": w_gu[:, :, 1::2], "w_dn": f(inp["w_dn"])[0],
        "b_glu": b_gu[:, 0::2], "b_lin": b_gu[:, 1::2], "b_dn": f(inp["b_dn"])[0],
    }
    shared = {k: np.ascontiguousarray(v, dtype=np.float32) for k, v in shared.items()}
    x = f(inp["x"])
    mem = f(inp["mem"])
    pos = np.ascontiguousarray(np.asarray(inp["positions"]), dtype=np.int32)
    maps = []
    for c in range(N_CORES):
        m = dict(shared)
        m["x"] = np.ascontiguousarray(x[c * NB:(c + 1) * NB].reshape(T, D))
        m["mem"] = np.ascontiguousarray(mem[c * NB:(c + 1) * NB].reshape(NB * 256, D))
        m["pos"] = np.ascontiguousarray(pos[c * NB:(c + 1) * NB])
        maps.append(m)
    return maps


def kernel(**inputs):
    maps = _host_layout(inputs)
    nc = build_program()
    res = run_bass_kernel_spmd(nc, maps, core_ids=list(range(N_CORES)))
    out = np.concatenate([np.asarray(r["out"]).reshape(NB, S, D) for r in res.results], axis=0)
    return out.astype(np.float32)
```

```python
import contextlib
import os
import numpy as np
import concourse.bass as bass
import concourse.mybir as mybir
from concourse.bass_utils import run_bass_kernel_spmd

F32 = mybir.dt.float32
BF16 = mybir.dt.bfloat16
I32 = mybir.dt.int32
AF = mybir.ActivationFunctionType
ALU = mybir.AluOpType

ENGS = ("pe", "act", "dve", "pool", "sp")

N_CORES = 8
NB = 2
S = 2048
T = NB * S
D = 1024
NG = 4
CAP = 640
NE = 32
NSLOT = NE * CAP
DN_ALPHA = 2.0 ** 0.25
EPS = 1e-5
BIG = 1.0e6


class _Op:
    __slots__ = ("eng", "fn", "deps", "is_dma", "tile", "dma_val", "signal", "sig_val")


class Prog:
    def __init__(self, nc):
        self.nc = nc
        self.ops = {e: [] for e in ENGS}
        self.last_w = {}
        self.readers = {}
        self.dma_cnt = {}
        self.dma_tiles = []
        self.n = 0
        self.limit = int(os.environ.get("KCUT", "0")) or None

    def mark(self, label):
        if os.environ.get("KDBG"):
            print("MARK", label, self.n)

    def op(self, eng, fn, reads=(), writes=(), dma=False):
        self.n += 1
        if self.limit is not None and self.n > self.limit and fn is not None:
            fn = None
            dma = False
            reads, writes = (), ()
        reads = [k[:3] if k.startswith("ps") else k for k in reads]
        writes = [k[:3] if k.startswith("ps") else k for k in writes]
        writes = writes + [k for k in reads if k.startswith("ps") and k not in writes]
        o = _Op()
        o.eng, o.fn, o.is_dma, o.signal, o.sig_val = eng, fn, dma, False, 0
        deps = []
        for t in reads:
            w = self.last_w.get(t)
            if w is not None:
                deps.append(w)
        for t in writes:
            w = self.last_w.get(t)
            if w is not None and not (dma and w.is_dma and w.tile == writes[0]):
                deps.append(w)
            deps.extend(self.readers.get(t, ()))
        if dma:
            t0 = writes[0]
            if t0 not in self.dma_cnt:
                self.dma_cnt[t0] = 0
                self.dma_tiles.append(t0)
            self.dma_cnt[t0] += 1
            o.tile, o.dma_val = t0, 16 * self.dma_cnt[t0]
        else:
            o.tile, o.dma_val = None, 0
        dd = []
        for d in deps:
            if d is o:
                continue
            if (not d.is_dma) and (not dma) and d.eng == eng == "pe":
                continue
            dd.append(d)
        o.deps = dd
        for t in reads:
            self.readers.setdefault(t, []).append(o)
        for t in writes:
            self.last_w[t] = o
            self.readers[t] = []
        self.ops[eng].append(o)
        return o

    def barrier_all(self):
        lasts = []
        for e in ENGS:
            comp = [o for o in self.ops[e] if not o.is_dma and o.fn is not None]
            if comp:
                lasts.append(comp[-1])
        dmas = {}
        for e in ENGS:
            for o in self.ops[e]:
                if o.is_dma:
                    dmas[o.tile] = o
        lasts.extend(dmas.values())
        for e in ENGS:
            o = self.op(e, None)
            o.deps = list(lasts)
        self.last_w = {}
        self.readers = {}

    def emit(self, final_tiles=()):
        nc = self.nc
        fin = self.op("sp", None)
        fin.deps = [self.last_w[t] for t in final_tiles if t in self.last_w]
        for e in ENGS:
            comp = [o for o in self.ops[e] if not o.is_dma and o.fn is not None]
            if comp:
                fin.deps.append(comp[-1])
        for e in ENGS:
            for o in self.ops[e]:
                for d in o.deps:
                    if not d.is_dma:
                        d.signal = True
        for e in ENGS:
            c = 0
            for o in self.ops[e]:
                if o.signal and not o.is_dma:
                    c += 1
                    o.sig_val = c
        with contextlib.ExitStack() as st:
            esem = {e: st.enter_context(nc.semaphore("s_" + e)) for e in ENGS}
            dsem = {}
            for i, t in enumerate(self.dma_tiles):
                dsem[t] = st.enter_context(nc.semaphore("d%d" % i))
            block = st.enter_context(nc.Block())

            def run(e):
                def body(eng):
                    known = {}
                    for o in self.ops[e]:
                        need = {}
                        for d in o.deps:
                            if d.is_dma:
                                key, val = ("d", d.tile), d.dma_val
                            else:
                                key, val = ("e", d.eng), d.sig_val
                            if val > need.get(key, 0):
                                need[key] = val
                        for key, val in need.items():
                            if known.get(key, 0) >= val:
                                continue
                            known[key] = val
                            sem = dsem[key[1]] if key[0] == "d" else esem[key[1]]
                            eng.wait_ge(sem, val)
                        if o.fn is None:
                            continue
                        ins = o.fn(eng)
                        if o.is_dma:
                            ins.then_inc(dsem[o.tile], 16)
                        elif o.signal:
                            ins.then_inc(esem[e], 1)
                return body

            block.tensor(run("pe"))
            block.scalar(run("act"))
            block.vector(run("dve"))
            block.gpsimd(run("pool"))
            block.sync(run("sp"))


class Arena:
    def __init__(self, tensor, nbytes):
        self.t, self.n, self.off = tensor, nbytes, 0

    def alloc(self, free_shape, dt, parts=128):
        esz = 2 if dt == BF16 else 4
        n = 1
        for s_ in free_shape:
            n *= s_
        nb = (n * esz + 63) // 64 * 64
        if self.off + nb > self.n:
            raise RuntimeError("arena overflow: need %d at %d of %d" % (nb, self.off, self.n))
        o = self.off // 2
        self.off += nb
        ap = self.t[:, o:o + n * esz // 2]
        if dt != BF16:
            ap = ap.bitcast(dt)
        if len(free_shape) == 2:
            ap = ap.rearrange("p (a b) -> p a b", a=free_shape[0])
        elif len(free_shape) == 3:
            ap = ap.rearrange("p (a b c) -> p a b c", a=free_shape[0], b=free_shape[1])
        if parts != 128:
            ap = ap[0:parts]
        return ap

    def mark(self):
        return self.off

    def release(self, m):
        self.off = m


def build_program(stage="full", debug=False, skip1=False):
    nc = bass.Bass("TRN2", target_bir_lowering=False)

    def din(name, shape, dt=F32):
        return nc.dram_tensor(name, list(shape), dt, kind="ExternalInput").ap()

    def dscr(name, shape, dt, dbg=False):
        kind = "ExternalOutput" if (debug and dbg) else "Internal"
        return nc.dram_tensor(name, list(shape), dt, kind=kind).ap()

    x_d = din("x", [T, D])
    mem_d = din("mem", [NB * 256, D])
    pos_d = din("pos", [NB, S], I32)
    wa_d = din("w_a", [D, 512])
    wb_d = din("w_b", [D, 1544])
    wc_d = din("w_c", [D, 2048])
    wuq_d = din("w_uq2", [256, 1536])
    wukv_d = din("w_ukv2", [128, 1024])
    wmqk_d = din("w_mqk", [128, 1024])
    pm_d = din("pm", [128, 32])
    gb_d = din("gb", [1, 8])
    lnp_d = din("lnp", [6, D])
    wbrm_d = din("w_br_m", [512, D])
    wbra_d = din("w_br_a", [512, D])
    wmix_d = din("w_mix", [D, D])
    wcq_d = din("w_cq", [D, D])
    wckv_d = din("w_ckv", [D, 2 * D])
    wco_d = din("w_co", [D, D])
    wr_d = din("w_router", [D, NE])
    br_d = din("b_router", [1, NE])
    wglu_d = din("w_glu", [NE, D, D])
    wlin_d = din("w_lin", [NE, D, D])
    wdn_d = din("w_dn", [NE, D, D])
    bglu_d = din("b_glu", [NE, D])
    blin_d = din("b_lin", [NE, D])
    bdn_d = din("b_dn", [NE, D])
    out_d = nc.dram_tensor("out", [T, D], F32, kind="ExternalOutput").ap()

    xT_d = dscr("xT_scr", [NB * NG, 128, 8 * 512], BF16)
    h1_d = din("h1_in", [T, D]) if skip1 else dscr("h1_scr", [T, D], F32, True)
    h2_d = dscr("h2_scr", [T, D], F32, True)
    dbg_d = dscr("dbg_scr", [128, 8 * S], BF16, True)
    dbg2_d = dscr("dbg2_scr", [128, 256], F32, True)
    xs_d = dscr("xs_scr", [NSLOT, D], BF16)
    ys_d = dscr("ys_scr", [NSLOT, D], BF16)

    ARENA_BYTES = 204 * 1024
    arena_t = nc.alloc_sbuf_tensor("arena", [128, ARENA_BYTES // 2], BF16)
    A = Arena(arena_t, ARENA_BYTES)
    psb = [nc.alloc_psum_tensor("ps%d" % i, [128, 512], F32) for i in range(8)]

    def PS(i, n=512, parts=128, c0=0):
        return psb[i][0:parts, c0:c0 + n]

    def PSB(i, n=1024, parts=128, c0=0):
        return psb[i][:, :].bitcast(BF16)[0:parts, c0:c0 + n]

    P = Prog(nc)
    _bc = {}

    def bc_reg(e):
        if "r" not in _bc:
            _bc["r"] = e.to_reg(NSLOT - 1)
        return _bc["r"]

    def OP(eng, method, reads, writes, *args, **kw):
        return P.op(eng, lambda e: getattr(e, method)(*args, **kw), reads=reads, writes=writes)

    def DMA(q, out, in_, reads, writes, **kw):
        return P.op(q, lambda e: e.dma_start(out=out, in_=in_, **kw), reads=reads, writes=writes, dma=True)

    def MM(out, lhsT, rhs, start, stop, reads, writes):
        return P.op("pe", lambda e: e.matmul(out, lhsT=lhsT, rhs=rhs, start=start, stop=stop),
                    reads=reads, writes=writes)

    def ACT(out, in_, func, reads, writes, **kw):
        return P.op("act", lambda e: e.activation(out=out, in_=in_, func=func, **kw), reads=reads, writes=writes)

    iota_f = A.alloc([512], F32)
    pidx = A.alloc([1], F32)
    tri_f = A.alloc([128], F32)
    stri_b = A.alloc([128], BF16)
    ident_b = A.alloc([128], BF16)
    ident_f = A.alloc([128], F32)
    ones_f = A.alloc([128], F32)
    ones_b = A.alloc([128], BF16)
    masks = A.alloc([4, 512], BF16)
    eps_c = A.alloc([1], F32)
    one_c = A.alloc([1], F32)
    pm = A.alloc([32], F32)
    gbt = A.alloc([8], F32)
    ropec = A.alloc([4], F32)
    CONST = ["const"]

    OP("pool", "iota", [], ["iota"], iota_f, [[1, 512]], base=0, channel_multiplier=-1,
       allow_small_or_imprecise_dtypes=True)
    OP("pool", "iota", [], ["pidx"], pidx, [[0, 1]], base=0, channel_multiplier=1,
       allow_small_or_imprecise_dtypes=True)
    OP("dve", "tensor_single_scalar", ["iota"], ["c1"], out=tri_f, in_=iota_f[:, 0:128], scalar=0.0, op=ALU.is_ge)
    OP("dve", "tensor_single_scalar", ["iota"], ["c2"], out=stri_b, in_=iota_f[:, 0:128], scalar=1.0, op=ALU.is_ge)
    OP("dve", "tensor_single_scalar", ["iota"], ["c3"], out=ident_b, in_=iota_f[:, 0:128], scalar=0.0, op=ALU.is_equal)
    OP("dve", "tensor_single_scalar", ["iota"], ["c4"], out=ident_f, in_=iota_f[:, 0:128], scalar=0.0, op=ALU.is_equal)
    for j in range(4):
        OP("dve", "tensor_single_scalar", ["iota"], ["c5%d" % j], out=masks[:, j, :], in_=iota_f,
           scalar=float(128 * j), op=ALU.is_ge)
    OP("pool", "memset", [], ["c6"], ones_f, 1.0)
    OP("pool", "memset", [], ["c7"], ones_b, 1.0)
    OP("pool", "memset", [], ["c8"], eps_c, EPS)
    OP("pool", "memset", [], ["c9"], one_c, 1.0)
    DMA("sp", pm, pm_d, [], ["pm"])
    DMA("sp", gbt, gb_d[0:1, :].broadcast_to([128, 8]), [], ["gbt"])
    OP("dve", "tensor_single_scalar", ["pidx"], ["rc2"], out=ropec[:, 2:3], in_=pidx, scalar=80.0, op=ALU.is_ge)
    OP("dve", "scalar_tensor_tensor", ["rc2", "pidx"], ["rc3"], out=ropec[:, 3:4], in0=ropec[:, 2:3], scalar=-16.0,
       in1=pidx, op0=ALU.mult, op1=ALU.add)
    OP("dve", "tensor_scalar", ["rc3"], ["rc3b"], out=ropec[:, 3:4], in0=ropec[:, 3:4], scalar1=-64.0, scalar2=None,
       op0=ALU.add)
    ACT(ropec[:, 0:1], ropec[:, 3:4], AF.Exp, ["rc3b"], ["rc0"], scale=-float(np.log(10000.0)) / 16.0)
    OP("dve", "tensor_scalar", ["rc0"], ["rc0b"], out=ropec[:, 0:1], in0=ropec[:, 0:1],
       scalar1=float(1.0 / (2.0 * np.pi)), scalar2=None, op0=ALU.mult)
    OP("dve", "tensor_scalar", ["rc2"], ["rc1"], out=ropec[:, 1:2], in0=ropec[:, 2:3], scalar1=2.0, scalar2=-1.0,
       op0=ALU.mult, op1=ALU.add)
    P.barrier_all()
    base_mark = A.mark()

    def rstd_from_var(var_ap, out_ap, tmp_ap, rk, wk):
        ACT(tmp_ap, var_ap, AF.Ln, rk, [wk + "_t"], bias=eps_c[0:var_ap.shape[0]])
        ACT(out_ap, tmp_ap, AF.Exp, [wk + "_t"], [wk], scale=-0.5)

    def layer_norm_tile(xres, psl, psr, lng, lnb, pre, outt, st12, mv, tmpc, key, rk_res, rk_ps):
        kp, ks = key + "pre", key + "sm"
        OP("dve", "scalar_tensor_tensor", rk_res + [rk_ps[0]], [kp], out=pre[:, 0:512], in0=xres[:, 0:512],
           scalar=DN_ALPHA, in1=psl, op0=ALU.mult, op1=ALU.add)
        OP("dve", "scalar_tensor_tensor", rk_res + [rk_ps[1], kp], [kp], out=pre[:, 512:1024], in0=xres[:, 512:1024],
           scalar=DN_ALPHA, in1=psr, op0=ALU.mult, op1=ALU.add)
        OP("dve", "bn_stats", [kp], [ks], out=st12[:, 0:6], in_=pre[:, 0:512])
        OP("dve", "bn_stats", [kp, ks], [ks], out=st12[:, 6:12], in_=pre[:, 512:1024])
        OP("dve", "bn_aggr", [ks], [ks], out=mv[:, 0:2], in_=st12[:, 0:12])
        ACT(tmpc, mv[:, 1:2], AF.Ln, [ks], [ks], bias=eps_c)
        ACT(mv[:, 2:3], tmpc, AF.Exp, [ks], [ks], scale=-0.5)
        OP("dve", "tensor_scalar", [kp, ks], [kp], out=pre, in0=pre,
           scalar1=mv[:, 0:1], scalar2=mv[:, 2:3], op0=ALU.subtract, op1=ALU.mult)
        OP("pool", "tensor_tensor", [kp, "lnp"], [kp], out=pre, in0=pre, in1=lng, op=ALU.mult)
        OP("pool", "tensor_tensor", [kp, "lnp"], [key + "out"], out=outt, in0=pre, in1=lnb, op=ALU.add)

    U = A.alloc([32768], BF16)
    attnT = A.alloc([4, S], BF16)
    ymT = A.alloc([4, S], BF16)
    p1_mark = A.mark()
    SC = float(96.0 ** -0.5)

    for b in range(0 if skip1 else NB):
        A.release(p1_mark)
        KT = U[:, 0:16384].rearrange("p (h s) -> p h s", h=8)
        VA = U[:, 16384:32768].rearrange("p (t h c) -> p t h c", t=16, h=8)
        w_a = A.alloc([8, 512], BF16)
        w_uq = A.alloc([2, 1536], BF16)
        w_ukv = A.alloc([1024], BF16)
        for k in range(8):
            DMA("pool", w_a[:, k, :], wa_d[k * 128:(k + 1) * 128, :], [], ["w_a"])
        for k in range(2):
            DMA("pool", w_uq[:, k, :], wuq_d[k * 128:(k + 1) * 128, :], [], ["w_uq"])
        DMA("pool", w_ukv, wukv_d, [], ["w_ukv"])
        xf = A.alloc([2, 1024], F32)
        xb = A.alloc([2, 1024], BF16)
        xT = A.alloc([8, 512], BF16)
        sq = A.alloc([3, 512], F32)
        lat = A.alloc([3, 512], F32)
        rsq = A.alloc([2, 512], F32)
        latn = A.alloc([3, 512], BF16)
        posi = A.alloc([512], I32)
        rp = A.alloc([6, 512], F32)
        rt = A.alloc([2, 512], F32)
        kpe = A.alloc([512], BF16)
        qT = A.alloc([8, 512], BF16)
        PT = A.alloc([4, 512], BF16)
        dsb = A.alloc([2, 512], F32)
        OP("pool", "memset", [], ["VA%d" % t for t in range(16)], U[:, 16384:32768], 1.0)
        if b == 0:
            zt = A.alloc([4, 1024], BF16)
            OP("pool", "memset", [], ["zt"], zt, 0.0)
            xs_v = xs_d.rearrange("(n j p) d -> n p j d", p=128, j=4)
            for n in range(NSLOT // 512):
                DMA("sp", xs_v[n], zt, ["zt"], ["xs_zero"])
        for g in range(NG):
            G = b * NG + g
            gs = slice(g * 512, (g + 1) * 512)
            for t in range(4):
                tt = G * 4 + t
                s2 = t % 2
                DMA("sp", xf[:, s2, :], x_d[tt * 128:(tt + 1) * 128, :], [], ["xf%d" % s2])
                ACT(xb[:, s2, :], xf[:, s2, :], AF.Copy, ["xf%d" % s2], ["xb%d" % s2])
                for k in range(8):
                    OP("pe", "transpose", ["xb%d" % s2], ["ps%d" % (6 + s2)], out=PSB(6 + s2, 128, c0=k * 128),
                       in_=xb[:, s2, k * 128:(k + 1) * 128], identity=ident_b)
                OP("dve", "tensor_copy", ["ps%d" % (6 + s2)], ["xT"], out=xT[:, :, t * 128:(t + 1) * 128],
                   in_=PSB(6 + s2).rearrange("p (k c) -> p k c", k=8))
            DMA("sp", xT_d[G].rearrange("p (k c) -> p k c", k=8), xT, ["xT"], ["xT_d%d" % G])
            P.mark('lat g%d' % g)
            for c in range(3):
                pb = c % 2
                for k in range(8):
                    MM(PS(pb), w_a[:, k, c * 128:(c + 1) * 128], xT[:, k, :], k == 0, k == 7, ["w_a", "xT"], ["ps%d" % pb])
                ACT(sq[:, c, :], PS(pb), AF.Square, ["ps%d" % pb], ["sq%d" % c])
                gcol = pm[:, 28 + c:29 + c]
                OP("dve", "tensor_scalar", ["ps%d" % pb, "pm"], ["lat%d" % c], out=lat[:, c, :], in0=PS(pb), scalar1=gcol,
                   scalar2=None, op0=ALU.mult)
            MM(PS(2), ones_f, sq[:, 0, :], True, False, ["sq0"], ["ps2"])
            MM(PS(2), ones_f, sq[:, 1, :], False, True, ["sq1"], ["ps2"])
            MM(PS(3), ones_f, sq[:, 2, :], True, True, ["sq2"], ["ps3"])
            for i_, (bank, nrm) in enumerate(((2, 1.0 / 256.0), (3, 1.0 / 128.0))):
                ACT(rsq[:, i_, :], PS(bank), AF.Ln, ["ps%d" % bank], ["rsq%d" % i_], scale=nrm, bias=eps_c)
                ACT(rsq[:, i_, :], rsq[:, i_, :], AF.Exp, ["rsq%d" % i_], ["rsq%d" % i_], scale=-0.5)
            for c in range(3):
                OP("pool", "tensor_tensor", ["lat%d" % c, "rsq%d" % (c // 2)], ["latn%d" % c], out=latn[:, c, :],
                   in0=lat[:, c, :], in1=rsq[:, c // 2, :], op=ALU.mult)
            P.mark('rope g%d' % g)
            R = slice(64, 96)
            DMA("sp", posi[R], pos_d[b, gs].partition_broadcast(32), [], ["posi"])
            OP("dve", "tensor_copy", ["posi"], ["rp0"], out=rp[R, 0, :], in_=posi[R])
            OP("dve", "tensor_scalar", ["rp0"], ["rp0"], out=rp[R, 0, :], in0=rp[R, 0, :], scalar1=ropec[R, 0:1],
               scalar2=None, op0=ALU.mult)
            OP("dve", "tensor_copy", ["rp0", "posi"], ["posi"], out=posi[R], in_=rp[R, 0, :])
            OP("dve", "tensor_copy", ["posi"], ["rp5"], out=rp[R, 5, :], in_=posi[R])
            OP("dve", "tensor_tensor", ["rp0", "rp5"], ["rp0"], out=rp[R, 0, :], in0=rp[R, 0, :], in1=rp[R, 5, :],
               op=ALU.subtract)
            ACT(rp[R, 1, :], rp[R, 0, :], AF.Sin, ["rp0"], ["rp1"], scale=float(np.pi))
            ACT(rp[R, 2, :], rp[R, 0, :], AF.Sin, ["rp0"], ["rp2"], scale=float(np.pi / 2.0))
            OP("dve", "tensor_tensor", ["rp1"], ["rp3"], out=rp[R, 3, :], in0=rp[R, 1, :], in1=rp[R, 1, :], op=ALU.mult)
            OP("dve", "tensor_scalar", ["rp3"], ["rp3"], out=rp[R, 3, :], in0=rp[R, 3, :], scalar1=-2.0, scalar2=1.0,
               op0=ALU.mult, op1=ALU.add)
            OP("dve", "tensor_tensor", ["rp2", "rp5"], ["rp5"], out=rp[R, 5, :], in0=rp[R, 2, :], in1=rp[R, 2, :], op=ALU.mult)
            OP("dve", "tensor_scalar", ["rp5"], ["rp5"], out=rp[R, 5, :], in0=rp[R, 5, :], scalar1=-4.0, scalar2=2.0,
               op0=ALU.mult, op1=ALU.add)
            OP("dve", "tensor_tensor", ["rp5", "rp1"], ["rp4"], out=rp[R, 4, :], in0=rp[R, 5, :], in1=rp[R, 1, :],
               op=ALU.mult)
            OP("dve", "tensor_scalar", ["rp4"], ["rp4"], out=rp[R, 4, :], in0=rp[R, 4, :], scalar1=ropec[R, 1:2],
               scalar2=None, op0=ALU.mult)
            cosR, sinR = rp[R, 3, :], rp[R, 4, :]

            def rope_evac(psA, psB, dst, keyA, keyB, wkey):
                OP("dve", "tensor_tensor", [keyA, "rp3"], ["rt0"], out=rt[R, 0, :], in0=psA, in1=cosR, op=ALU.mult)
                OP("dve", "tensor_tensor", [keyB, "rp4"], ["rt1"], out=rt[R, 1, :], in0=psB, in1=sinR, op=ALU.mult)
                OP("pool", "tensor_tensor", ["rt0", "rt1"], wkey, out=dst, in0=rt[R, 0, :], in1=rt[R, 1, :], op=ALU.add)

            P.mark('kpe g%d' % g)
            for k in range(8):
                MM(PS(0, parts=96), w_a[:, k, 320:416], xT[:, k, :], k == 0, k == 7, ["w_a", "xT"], ["ps0"])
            for k in range(8):
                MM(PS(1, parts=96), w_a[:, k, 416:512], xT[:, k, :], k == 0, k == 7, ["w_a", "xT"], ["ps1"])
            rope_evac(PS(0)[R], PS(1)[R], kpe[R], "ps0", "ps1", ["kpe"])
            for h in range(8):
                OP("pool", "tensor_copy", ["kpe"], ["KT%d" % h], out=KT[R, h, gs], in_=kpe[R])
            P.mark('knope g%d' % g)
            for h in range(8):
                pb = 2 + h % 2
                MM(PS(pb, parts=64), w_ukv[:, h * 64:(h + 1) * 64], latn[:, 2, :], True, True, ["w_ukv", "latn2"], ["ps%d" % pb])
                ACT(KT[0:64, h, gs], PS(pb, parts=64), AF.Copy, ["ps%d" % pb], ["KT%d" % h])
            P.mark('V g%d' % g)
            for t in range(4):
                kt = g * 4 + t
                pb = t % 2
                MM(PS(pb), latn[:, 2, t * 128:(t + 1) * 128], w_ukv[:, 512:1024], True, True, ["latn2", "w_ukv"], ["ps%d" % pb])
                psv = PS(pb).rearrange("p (a e c) -> p a e c", a=4, e=2)
                vav = VA[:, kt].rearrange("p (a e) c -> p a e c", e=2)
                ACT(vav[:, :, 0, 0:64], psv[:, :, 0, :], AF.Copy, ["ps%d" % pb], ["VA%d" % kt])
                OP("dve", "tensor_copy", ["ps%d" % pb], ["VA%d" % kt], out=vav[:, :, 1, 64:128], in_=psv[:, :, 1, :])
            P.mark('q g%d' % g)
            for h in range(8):
                for kk in range(2):
                    MM(PS(2, parts=96), w_uq[:, kk, h * 96:(h + 1) * 96], latn[:, kk, :], kk == 0, kk == 1,
                       ["w_uq", "latn%d" % kk], ["ps2"])
                for kk in range(2):
                    MM(PS(3, parts=96), w_uq[:, kk, 768 + h * 96:768 + (h + 1) * 96], latn[:, kk, :], kk == 0, kk == 1,
                       ["w_uq", "latn%d" % kk], ["ps3"])
                ACT(qT[0:64, h, :], PS(2, parts=64), AF.Copy, ["ps2"], ["qT%d" % h])
                rope_evac(PS(2)[R], PS(3)[R], qT[R, h, :], "ps2", "ps3", ["qT%d" % h])
            P.mark('attn g%d' % g)
            nck = 4 * g + 4
            for h in range(8):
                ob = 4 + h % 2
                pr, hi = h // 2, h % 2

                def s_mm(c):
                    MM(PS(c % 4), KT[0:96, h, c * 128:(c + 1) * 128], qT[0:96, h, :], True, True,
                       ["KT%d" % h, "qT%d" % h], ["ps%d" % (c % 4)])
                s_mm(0)
                for c in range(nck):
                    if c + 1 < nck:
                        s_mm(c + 1)
                    pt = c % 4
                    ACT(PT[:, pt, :], PS(c % 4), AF.Exp, ["ps%d" % (c % 4)], ["PT%d" % pt], scale=SC)
                    if c >= 4 * g:
                        OP("pool", "tensor_tensor", ["PT%d" % pt], ["PT%d" % pt], out=PT[:, pt, :], in0=PT[:, pt, :],
                           in1=masks[:, c - 4 * g, :], op=ALU.mult)
                    MM(PS(ob), VA[:, c, h, :], PT[:, pt, :], c == 0, c == nck - 1, ["VA%d" % c, "PT%d" % pt], ["ps%d" % ob])
                lo, up = (slice(0, 64), slice(64, 128)) if hi == 0 else (slice(64, 128), slice(0, 64))
                ACT(dsb[up, 0, :], PS(ob)[up], AF.Copy, ["ps%d" % ob], ["dsb0"])
                OP("dve", "reciprocal", ["dsb0"], ["dsb1"], out=dsb[lo, 1, :], in_=dsb[up, 0, :])
                OP("dve", "tensor_tensor", ["ps%d" % ob, "dsb1"], ["attnT"], out=attnT[lo, pr, gs], in0=PS(ob)[lo],
                   in1=dsb[lo, 1, :], op=ALU.mult)
        P.barrier_all()
        if stage == "1a":
            DMA("sp", dbg_d[:, 0:4 * S].rearrange("p (k c) -> p k c", k=4), attnT, [], ["dbg"])
            P.emit(final_tiles=["dbg"])
            return nc

        A.release(p1_mark)
        w_b = A.alloc([8, 1544], BF16)
        w_mqk = A.alloc([1024], BF16)
        for k in range(8):
            DMA("pool", w_b[:, k, :], wb_d[k * 128:(k + 1) * 128, :], [], ["w_b"])
        DMA("pool", w_mqk, wmqk_d, [], ["w_mqk"])
        w_c = U[:, 0:16384].rearrange("p (k c) -> p k c", k=8)
        w_brm = U[:, 16384:20480].rearrange("p (k c) -> p k c", k=4)
        w_bra = U[:, 20480:24576].rearrange("p (k c) -> p k c", k=4)
        w_mix = U[:, 24576:32768].rearrange("p (k c) -> p k c", k=8)
        for k in range(8):
            DMA("pool", w_c[:, k, :], wc_d[k * 128:(k + 1) * 128, :], [], ["w_c"])
            DMA("pool", w_mix[:, k, :], wmix_d[k * 128:(k + 1) * 128, :], [], ["w_mix"])
        for k in range(4):
            DMA("pool", w_brm[:, k, :], wbrm_d[k * 128:(k + 1) * 128, :], [], ["w_brm"])
            DMA("pool", w_bra[:, k, :], wbra_d[k * 128:(k + 1) * 128, :], [], ["w_bra"])
        xT = A.alloc([8, 512], BF16)
        xme = A.alloc([4, 520], F32)
        acc = A.alloc([512], F32)
        xcf = A.alloc([4, 512], F32)
        xcb = A.alloc([4, 512], BF16)
        so = A.alloc([4, 512], BF16)
        mq = A.alloc([4, 512], BF16)
        mk = A.alloc([4, 512], BF16)
        vam = A.alloc([4, 130], BF16)
        gt = A.alloc([16], F32)
        gtmp = A.alloc([8], F32)
        lfrep = A.alloc([128], F32)
        DTt = A.alloc([128], F32)
        EB = A.alloc([128], F32)
        DTm = A.alloc([128], F32)
        STb = A.alloc([128], BF16)
        qsb = A.alloc([128], BF16)
        kwb = A.alloc([128], BF16)
        Cf = A.alloc([4, 130], F32)
        Cb = A.alloc([4, 130], BF16)
        sml = A.alloc([16], F32)
        hv = A.alloc([128], F32)
        hnb = A.alloc([128], BF16)
        t1 = A.alloc([128], F32)
        t2 = A.alloc([128], F32)
        sml2 = A.alloc([2, 16], F32)
        hv2 = A.alloc([2, 128], F32)
        hnb2 = A.alloc([2, 128], BF16)
        t12 = A.alloc([2, 128], F32)
        t22 = A.alloc([2, 128], F32)

        def mlstm_second(h, ts_, ss):
            q2 = h % 2
            hb = (5, 1)[q2]
            sm = sml2[:, q2, :]
            k_ = "m2_%d" % q2
            hv_, hnb_, t1_, t2_ = hv2[:, q2, :], hnb2[:, q2, :], t12[:, q2, :], t22[:, q2, :]
            OP("dve", "tensor_copy", ["ps%d" % hb], [k_], out=sm[:, 0:1], in_=PS(hb, 1, c0=128))
            OP("dve", "scalar_tensor_tensor", [k_], [k_], out=sm[:, 1:2], in0=sm[:, 0:1], scalar=-1.0,
               in1=sm[:, 0:1], op0=ALU.mult, op1=ALU.max)
            OP("dve", "tensor_scalar", [k_], [k_], out=sm[:, 2:3], in0=sm[:, 1:2], scalar1=1.0, scalar2=None, op0=ALU.max)
            OP("dve", "reciprocal", [k_], [k_], out=sm[:, 3:4], in_=sm[:, 2:3])
            ACT(hv_, PS(hb, 128), AF.Copy, ["ps%d" % hb, k_], [k_ + "hv"], scale=sm[:, 3:4])
            OP("dve", "bn_stats", [k_ + "hv"], [k_], out=sm[:, 4:10], in_=hv_)
            OP("dve", "bn_aggr", [k_], [k_], out=sm[:, 10:12], in_=sm[:, 4:10])
            ACT(sm[:, 13:14], sm[:, 11:12], AF.Ln, [k_], [k_], bias=eps_c)
            ACT(sm[:, 12:13], sm[:, 13:14], AF.Exp, [k_], [k_], scale=-0.5)
            OP("dve", "tensor_scalar", [k_ + "hv", k_], [k_ + "hn"], out=hnb_, in0=hv_, scalar1=sm[:, 10:11],
               scalar2=sm[:, 12:13], op0=ALU.subtract, op1=ALU.mult)
            OP("pe", "transpose", [k_ + "hn"], ["ps7"], out=PSB(7, 128), in_=hnb_, identity=ident_b)
            OP("dve", "tensor_scalar", ["ps7", "pm"], [k_ + "t1"], out=t1_, in0=PSB(7, 128), scalar1=pm[:, 20 + h:21 + h],
               scalar2=None, op0=ALU.mult)
            OP("dve", "scalar_tensor_tensor", ["xcf%d" % h, "pm", k_ + "t1"], [k_ + "t2"], out=t2_, in0=xcf[:, h, ts_],
               scalar=pm[:, 24 + h:25 + h], in1=t1_, op0=ALU.mult, op1=ALU.add)
            OP("pool", "tensor_tensor", [k_ + "t2", "so%d" % h], ["ymT"], out=ymT[:, h, ss], in0=t2_, in1=so[:, h, ts_],
               op=ALU.mult)

        pend_m = None
        OP("pool", "memset", [], ["Cf%d" % h for h in range(4)], Cf, 0.0)
        OP("pool", "memset", [], ["Cb%d" % h for h in range(4)], Cb, 0.0)
        OP("pool", "memset", [], ["xme%d" % c for c in range(4)], xme, 0.0)
        OP("pool", "memset", [], ["vam"], vam, 1.0)
        DSC = float(128.0 ** -0.5)
        for g in range(NG):
            G = b * NG + g
            gs = slice(g * 512, (g + 1) * 512)
            DMA("sp", xT, xT_d[G].rearrange("p (k c) -> p k c", k=8), ["xT_d%d" % G], ["xT"])
            for c in range(4):
                pb = c % 2
                for k in range(8):
                    MM(PS(pb), w_b[:, k, c * 128:(c + 1) * 128], xT[:, k, :], k == 0, k == 7, ["w_b", "xT"], ["ps%d" % pb])
                ACT(xme[:, c, 3:515], PS(pb), AF.Copy, ["ps%d" % pb], ["xme%d" % c])
                OP("dve", "tensor_scalar", ["xme%d" % c, "pm"], ["acc"], out=acc, in0=xme[:, c, 0:512],
                   scalar1=pm[:, c * 4:c * 4 + 1], scalar2=None, op0=ALU.mult)
                for j in range(1, 4):
                    OP("dve", "scalar_tensor_tensor", ["xme%d" % c, "pm", "acc"], ["acc"], out=acc, in0=xme[:, c, j:j + 512],
                       scalar=pm[:, c * 4 + j:c * 4 + j + 1], in1=acc, op0=ALU.mult, op1=ALU.add)
                ACT(xcf[:, c, :], acc, AF.Silu, ["acc", "pm"], ["xcf%d" % c], bias=pm[:, 16 + c:17 + c])
                OP("pool", "tensor_copy", ["xcf%d" % c], ["xcb%d" % c], out=xcb[:, c, :], in_=xcf[:, c, :])
                OP("pool", "tensor_copy", ["xme%d" % c], ["xme%d" % c], out=xme[:, c, 0:3], in_=xme[:, c, 512:515])
            for c in range(4):
                pb = c % 2
                for k in range(8):
                    MM(PS(pb), w_b[:, k, 1024 + c * 128:1024 + (c + 1) * 128], xT[:, k, :], k == 0, k == 7,
                       ["w_b", "xT"], ["ps%d" % pb])
                ACT(so[:, c, :], PS(pb), AF.Sigmoid, ["ps%d" % pb], ["so%d" % c])
            for h in range(4):
                MM(PS(0), w_mqk[:, h * 128:(h + 1) * 128], xcb[:, h, :], True, True, ["w_mqk", "xcb%d" % h], ["ps0"])
                ACT(mq[:, h, :], PS(0), AF.Copy, ["ps0"], ["mq%d" % h])
                MM(PS(1), w_mqk[:, 512 + h * 128:512 + (h + 1) * 128], xcb[:, h, :], True, True, ["w_mqk", "xcb%d" % h], ["ps1"])
                ACT(mk[:, h, :], PS(1), AF.Copy, ["ps1"], ["mk%d" % h], scale=DSC)
            for t in range(4):
                ts_ = slice(t * 128, (t + 1) * 128)
                ss = slice(g * 512 + t * 128, g * 512 + (t + 1) * 128)
                for k in range(8):
                    MM(PS(0), xT[:, k, ts_], w_b[:, k, 512:1024], k == 0, k == 7, ["xT", "w_b"], ["ps0"])
                for k in range(8):
                    MM(PS(2, 8, c0=256), xT[:, k, ts_], w_b[:, k, 1536:1544], k == 0, k == 7, ["xT", "w_b"], ["ps2c"])
                ACT(vam[:, :, 0:128], PS(0).rearrange("p (h c) -> p h c", h=4), AF.Copy, ["ps0"], ["vam"])
                OP("dve", "tensor_tensor", ["ps2c", "gbt"], ["gt"], out=gt[:, 0:8], in0=PS(2, 8, c0=256), in1=gbt, op=ALU.add)
                ACT(gtmp[:, 0:4], gt[:, 4:8], AF.Exp, ["gt"], ["gtmp"], scale=-1.0)
                ACT(gtmp[:, 4:8], gtmp[:, 0:4], AF.Ln, ["gtmp"], ["gtmp2"], bias=one_c)
                OP("dve", "tensor_scalar", ["gtmp2"], ["lf"], out=gt[:, 8:12], in0=gtmp[:, 4:8], scalar1=-1.0, scalar2=None,
                   op0=ALU.mult)
                MM(PS(2, 4, c0=128), tri_f, gt[:, 8:12], True, True, ["lf"], ["ps2b"])
                OP("dve", "tensor_tensor", ["gt", "ps2b"], ["ccol"], out=gt[:, 12:16], in0=gt[:, 0:4], in1=PS(2, 4, c0=128),
                   op=ALU.subtract)
                for h in range(4):
                    OP("dve", "tensor_scalar", ["lf"], ["lfrep"], out=lfrep, in0=ones_f, scalar1=gt[:, 8 + h:9 + h],
                       scalar2=None, op0=ALU.mult)
                    MM(PS(2, 128), lfrep, tri_f, True, True, ["lfrep"], ["ps2a"])
                    ACT(DTt, PS(2, 128), AF.Exp, ["ps2a", "ccol"], ["DT"], bias=gt[:, 12 + h:13 + h])
                    ACT(EB, PS(2, 128), AF.Exp, ["ps2a"], ["EB"])
                    OP("pool", "tensor_tensor", ["DT"], ["DTm"], out=DTm, in0=DTt, in1=tri_f, op=ALU.mult)
                    MM(PS(3, 128), mk[:, h, ts_], mq[:, h, ts_], True, True, ["mk%d" % h, "mq%d" % h], ["ps3"])
                    OP("dve", "tensor_tensor", ["ps3", "DTm"], ["STb"], out=STb, in0=PS(3, 128), in1=DTm, op=ALU.mult)
                    OP("pool", "tensor_tensor", ["mq%d" % h, "EB"], ["qsb"], out=qsb, in0=mq[:, h, ts_], in1=EB, op=ALU.mult)
                    MM(PS(4, 128), xcb[:, h, ts_], w_mqk[:, 512 + h * 128:512 + (h + 1) * 128], True, True,
                       ["xcb%d" % h, "w_mqk"], ["ps4"])
                    OP("dve", "tensor_scalar", ["ps4", "DT"], ["kwb"], out=kwb, in0=PS(4, 128), scalar1=DTt[:, 127:128],
                       scalar2=DSC, op0=ALU.mult, op1=ALU.mult)
                    hb = (5, 1)[h % 2]
                    MM(PS(hb, 129), STb, vam[:, h, 0:129], True, False, ["STb", "vam"], ["ps%d" % hb])
                    MM(PS(hb, 129), qsb, Cb[:, h, 0:129], False, True, ["qsb", "Cb%d" % h], ["ps%d" % hb])
                    MM(PS(6, 129), kwb, vam[:, h, 0:129], True, True, ["kwb", "vam"], ["ps6"])
                    OP("dve", "scalar_tensor_tensor", ["Cf%d" % h, "EB", "ps6"], ["Cf%d" % h], out=Cf[:, h, 0:129],
                       in0=Cf[:, h, 0:129], scalar=EB[:, 127:128], in1=PS(6, 129), op0=ALU.mult, op1=ALU.add)
                    ACT(Cb[:, h, 0:129], Cf[:, h, 0:129], AF.Copy, ["Cf%d" % h], ["Cb%d" % h])
                    if pend_m is not None:
                        mlstm_second(*pend_m)
                    pend_m = (h, ts_, ss)
            if pend_m is not None:
                mlstm_second(*pend_m)
                pend_m = None
        P.barrier_all()
        if stage == "1b":
            DMA("sp", dbg_d[:, 0:4 * S].rearrange("p (k c) -> p k c", k=4), attnT, [], ["dbg"])
            DMA("sp", dbg_d[:, 4 * S:8 * S].rearrange("p (k c) -> p k c", k=4), ymT, [], ["dbg"])
            P.emit(final_tiles=["dbg"])
            return nc

        A.release(p1_mark)
        xT = A.alloc([8, 512], BF16)
        sg = A.alloc([16, 512], BF16)
        ma = A.alloc([2, 512], F32)
        mixin = A.alloc([8, 512], BF16)
        lng = A.alloc([1024], F32)
        lnb = A.alloc([1024], F32)
        xf = A.alloc([2, 1024], F32)
        pre = A.alloc([2, 1024], F32)
        h1t = A.alloc([2, 1024], F32)
        lsm = A.alloc([2, 16], F32)
        DMA("sp", lng, lnp_d[0:1, :].broadcast_to([128, 1024]), [], ["lnp"])
        DMA("sp", lnb, lnp_d[1:2, :].broadcast_to([128, 1024]), [], ["lnp"])
        for g in range(NG):
            G = b * NG + g
            gs = slice(g * 512, (g + 1) * 512)
            DMA("sp", xT, xT_d[G].rearrange("p (k c) -> p k c", k=8), ["xT_d%d" % G], ["xT"])
            for c in range(16):
                pb = c % 2
                for k in range(8):
                    MM(PS(pb), w_c[:, k, c * 128:(c + 1) * 128], xT[:, k, :], k == 0, k == 7, ["w_c", "xT"], ["ps%d" % pb])
                ACT(sg[:, c, :], PS(pb), AF.Sigmoid, ["ps%d" % pb], ["sg%d" % c])
            for c in range(8):
                for kk in range(4):
                    MM(PS(2), w_brm[:, kk, c * 128:(c + 1) * 128], ymT[:, kk, gs], kk == 0, kk == 3, ["w_brm", "ymT"], ["ps2"])
                for kk in range(4):
                    MM(PS(3), w_bra[:, kk, c * 128:(c + 1) * 128], attnT[:, kk, gs], kk == 0, kk == 3, ["w_bra", "attnT"], ["ps3"])
                OP("dve", "tensor_tensor", ["ps2", "sg%d" % c], ["ma0"], out=ma[:, 0, :], in0=PS(2), in1=sg[:, c, :], op=ALU.mult)
                OP("dve", "tensor_tensor", ["ps3", "sg%d" % (8 + c)], ["ma1"], out=ma[:, 1, :], in0=PS(3), in1=sg[:, 8 + c, :],
                   op=ALU.mult)
                OP("pool", "tensor_tensor", ["ma0", "ma1"], ["mixin"], out=mixin[:, c, :], in0=ma[:, 0, :], in1=ma[:, 1, :],
                   op=ALU.add)
            for t in range(4):
                tt = G * 4 + t
                s2 = t % 2
                DMA("sp", xf[:, s2, :], x_d[tt * 128:(tt + 1) * 128, :], [], ["xf%d" % s2])
                for half in range(2):
                    for k in range(8):
                        MM(PS(4 + half), mixin[:, k, t * 128:(t + 1) * 128], w_mix[:, k, half * 512:(half + 1) * 512],
                           k == 0, k == 7, ["mixin", "w_mix"], ["ps%d" % (4 + half)])
                layer_norm_tile(xf[:, s2, :], PS(4), PS(5), lng, lnb, pre[:, s2, :], h1t[:, s2, :], lsm[:, s2, 0:12],
                                lsm[:, s2, 12:15], lsm[:, s2, 15:16], "ln%d" % s2, ["xf%d" % s2], ["ps4", "ps5"])
                DMA("sp", h1_d[tt * 128:(tt + 1) * 128, :], h1t[:, s2, :], ["ln%dout" % s2], ["h1_d%d" % s2])
        P.barrier_all()
        if stage == "1c":
            P.emit(final_tiles=[])
            return nc

    if stage == "1":
        P.emit(final_tiles=[])
        return nc
    A.release(base_mark)
    g4_all = A.alloc([T // 128, 4], F32)
    slot_all = A.alloc([T // 128, 4], I32)
    keep_mark = A.mark()
    w_cq = A.alloc([8, 1024], BF16)
    w_co = A.alloc([8, 1024], BF16)
    w_ckv = A.alloc([8, 2048], BF16)
    w_rt = A.alloc([8, 32], F32)
    brt = A.alloc([32], F32)
    lng = A.alloc([1024], F32)
    lnb = A.alloc([1024], F32)
    for k in range(8):
        DMA("pool", w_cq[:, k, :], wcq_d[k * 128:(k + 1) * 128, :], [], ["w_cq"])
        DMA("pool", w_co[:, k, :], wco_d[k * 128:(k + 1) * 128, :], [], ["w_co"])
        DMA("pool", w_ckv[:, k, :], wckv_d[k * 128:(k + 1) * 128, :], [], ["w_ckv"])
    DMA("sp", w_rt, wr_d.rearrange("(k p) e -> p k e", p=128), [], ["w_rt"])
    DMA("sp", brt, br_d[0:1, :].broadcast_to([128, NE]), [], ["brt"])
    DMA("sp", lng, lnp_d[2:3, :].broadcast_to([128, 1024]), [], ["lnp"])
    DMA("sp", lnb, lnp_d[3:4, :].broadcast_to([128, 1024]), [], ["lnp"])
    memf = A.alloc([1024], F32)
    memb = A.alloc([1024], BF16)
    memT = A.alloc([8, 256], BF16)
    KmT = A.alloc([8, 256], BF16)
    Vm = A.alloc([2, 1024], BF16)
    h1f = A.alloc([4, 1024], F32)
    h1b = A.alloc([1024], BF16)
    h1T = A.alloc([8, 512], BF16)
    qcT = A.alloc([8, 512], BF16)
    PT2 = A.alloc([2, 512], BF16)
    rden = A.alloc([512], F32)
    oT = A.alloc([8, 512], BF16)
    pre = A.alloc([2, 1024], F32)
    h2t = A.alloc([2, 1024], F32)
    h2b = A.alloc([2, 1024], BF16)
    lsm = A.alloc([2, 16], F32)
    h2T = A.alloc([8, 128], F32)
    lg = A.alloc([32], F32)
    m8 = A.alloc([8], F32)
    rsm = A.alloc([16], F32)
    maskb = A.alloc([32], BF16)
    if skip1:
        zt = A.alloc([4, 1024], BF16)
        OP("pool", "memset", [], ["zt"], zt, 0.0)
        xs_v = xs_d.rearrange("(n j p) d -> n p j d", p=128, j=4)
        for n in range(NSLOT // 512):
            DMA("sp", xs_v[n], zt, ["zt"], ["xs_zero"])
    carry = A.alloc([32], F32)
    lim = A.alloc([32], F32)
    slotm = A.alloc([32], F32)
    ovf = A.alloc([32], F32)
    oh = A.alloc([32], F32)
    s4f = A.alloc([4], F32)
    OP("pool", "iota", [], ["carry"], carry, [[CAP, NE]], base=0, channel_multiplier=0, allow_small_or_imprecise_dtypes=True)
    OP("pool", "iota", [], ["lim"], lim, [[CAP, NE]], base=CAP, channel_multiplier=0, allow_small_or_imprecise_dtypes=True)
    def router_tile(tt, s2):
        P.mark('p2 router %d' % tt)
        for k in range(8):
            MM(PS(4 + k // 4, 128, c0=(k % 4) * 128), h2t[:, s2, k * 128:(k + 1) * 128], ident_f, True, True,
               ["l2%dout" % s2], ["ps%d" % (4 + k // 4)])
        for hh in range(2):
            OP("dve", "tensor_copy", ["ps%d" % (4 + hh)], ["h2T"], out=h2T[:, hh * 4:(hh + 1) * 4, :],
               in_=PS(4 + hh).rearrange("p (k c) -> p k c", k=4))
        for k in range(8):
            MM(PS(2, 32), h2T[:, k, :], w_rt[:, k, :], k == 0, k == 7, ["h2T", "w_rt"], ["ps2"])
        OP("dve", "tensor_tensor", ["ps2", "brt"], ["lg"], out=lg, in0=PS(2, 32), in1=brt, op=ALU.add)
        OP("dve", "max", ["lg"], ["m8"], out=m8, in_=lg)
        OP("dve", "tensor_scalar", ["m8"], ["rs0"], out=rsm[:, 0:1], in0=m8[:, 0:1], scalar1=-1.0, scalar2=None,
           op0=ALU.mult)
        ACT(rsm[:, 4:8], m8[:, 0:4], AF.Exp, ["m8", "rs0"], ["rs4"], bias=rsm[:, 0:1], accum_out=rsm[:, 1:2])
        OP("dve", "reciprocal", ["rs4"], ["rs2"], out=rsm[:, 2:3], in_=rsm[:, 1:2])
        OP("dve", "tensor_scalar", ["rs4", "rs2"], ["g4"], out=g4_all[:, tt, :], in0=rsm[:, 4:8], scalar1=rsm[:, 2:3],
           scalar2=None, op0=ALU.mult)
        OP("dve", "tensor_scalar", ["lg", "m8"], ["maskb"], out=maskb, in0=lg, scalar1=m8[:, 3:4], scalar2=None,
           op0=ALU.is_ge)
        MM(PS(3, 32), stri_b, maskb, True, True, ["maskb"], ["ps3"])
        MM(PS(3, 32, c0=64), ones_b, maskb, True, True, ["maskb"], ["ps3"])
        OP("dve", "tensor_tensor", ["ps3", "carry"], ["slotm"], out=slotm, in0=PS(3, 32), in1=carry, op=ALU.add)
        OP("dve", "tensor_tensor", ["ps3", "carry"], ["carry"], out=carry, in0=PS(3, 32, c0=64), in1=carry, op=ALU.add)
        OP("dve", "tensor_tensor", ["slotm", "lim"], ["ovf"], out=ovf, in0=slotm, in1=lim, op=ALU.is_ge)
        OP("dve", "scalar_tensor_tensor", ["ovf", "slotm"], ["slotm"], out=slotm, in0=ovf, scalar=BIG, in1=slotm,
           op0=ALU.mult, op1=ALU.add)
        for k4 in range(4):
            OP("dve", "tensor_scalar", ["lg", "m8"], ["oh"], out=oh, in0=lg, scalar1=m8[:, k4:k4 + 1], scalar2=None,
               op0=ALU.is_equal)
            OP("dve", "tensor_tensor", ["oh", "slotm"], ["oh"], out=oh, in0=oh, in1=slotm, op=ALU.mult)
            OP("dve", "reduce_sum", ["oh"], ["s4f"], out=s4f[:, k4:k4 + 1], in_=oh, axis=mybir.AxisListType.X)
        OP("dve", "tensor_copy", ["s4f"], ["slot"], out=slot_all[:, tt, :], in_=s4f)
        OP("dve", "tensor_single_scalar", ["s4f"], ["s4f"], out=s4f, in_=s4f, scalar=float(NSLOT), op=ALU.is_lt)
        OP("dve", "tensor_tensor", ["s4f", "g4"], ["g4"], out=g4_all[:, tt, :], in0=g4_all[:, tt, :], in1=s4f, op=ALU.mult)
        P.mark('p2 scatter %d' % tt)
        for k4 in range(4):
            P.op("pool", (lambda e, tt=tt, k4=k4, s2=s2: e.indirect_dma_start(
                out=xs_d, out_offset=bass.IndirectOffsetOnAxis(ap=slot_all[:, tt, k4:k4 + 1], axis=0),
                in_=h2b[:, s2, :], in_offset=None, bounds_check=bc_reg(e), oob_is_err=False)),
                reads=["h2b%d" % s2, "slot", "xs_zero"], writes=["xs_w%d" % s2], dma=True)

    pend_r = None
    P.mark('p2 start')
    for b in range(NB):
        P.mark('p2 mem b%d' % b)
        for mt in range(2):
            DMA("sp", memf, mem_d[b * 256 + mt * 128:b * 256 + (mt + 1) * 128, :], [], ["memf"])
            ACT(memb, memf, AF.Copy, ["memf"], ["memb"])
            for k in range(8):
                OP("pe", "transpose", ["memb"], ["ps6"], out=PSB(6, 128, c0=k * 128), in_=memb[:, k * 128:(k + 1) * 128],
                   identity=ident_b)
            OP("dve", "tensor_copy", ["ps6"], ["memT"], out=memT[:, :, mt * 128:(mt + 1) * 128],
               in_=PSB(6).rearrange("p (k c) -> p k c", k=8))
        for c in range(8):
            pb = c % 2
            for k in range(8):
                MM(PS(pb, 256), w_ckv[:, k, c * 128:(c + 1) * 128], memT[:, k, :], k == 0, k == 7, ["w_ckv", "memT"], ["ps%d" % pb])
            ACT(KmT[:, c, :], PS(pb, 256), AF.Copy, ["ps%d" % pb], ["KmT"])
        for mt in range(2):
            for half in range(2):
                pb = half
                for k in range(8):
                    MM(PS(pb), memT[:, k, mt * 128:(mt + 1) * 128], w_ckv[:, k, 1024 + half * 512:1024 + (half + 1) * 512],
                       k == 0, k == 7, ["memT", "w_ckv"], ["ps%d" % pb])
                ACT(Vm[:, mt, half * 512:(half + 1) * 512], PS(pb), AF.Copy, ["ps%d" % pb], ["Vm"])
        for g in range(NG):
            G = b * NG + g
            P.mark('p2 grp %d' % G)
            for t in range(4):
                tt = G * 4 + t
                DMA("sp", h1f[:, t, :], h1_d[tt * 128:(tt + 1) * 128, :], [], ["h1f%d" % t])
                ACT(h1b, h1f[:, t, :], AF.Copy, ["h1f%d" % t], ["h1b"])
                s2 = t % 2
                for k in range(8):
                    OP("pe", "transpose", ["h1b"], ["ps%d" % (6 + s2)], out=PSB(6 + s2, 128, c0=k * 128),
                       in_=h1b[:, k * 128:(k + 1) * 128], identity=ident_b)
                OP("dve", "tensor_copy", ["ps%d" % (6 + s2)], ["h1T"], out=h1T[:, :, t * 128:(t + 1) * 128],
                   in_=PSB(6 + s2).rearrange("p (k c) -> p k c", k=8))
            for c in range(8):
                pb = c % 2
                for k in range(8):
                    MM(PS(pb), w_cq[:, k, c * 128:(c + 1) * 128], h1T[:, k, :], k == 0, k == 7, ["w_cq", "h1T"], ["ps%d" % pb])
                ACT(qcT[:, c, :], PS(pb), AF.Copy, ["ps%d" % pb], ["qcT"])
            P.mark('p2 attn %d' % G)
            for h in range(4):
                for mt in range(2):
                    for kk in range(2):
                        MM(PS(mt), KmT[:, 2 * h + kk, mt * 128:(mt + 1) * 128], qcT[:, 2 * h + kk, :], kk == 0, kk == 1,
                           ["KmT", "qcT"], ["ps%d" % mt])
                    ACT(PT2[:, mt, :], PS(mt), AF.Exp, ["ps%d" % mt], ["PT2%d" % mt], scale=1.0 / 16.0)
                for mt in range(2):
                    MM(PS(2), ones_b, PT2[:, mt, :], mt == 0, mt == 1, ["PT2%d" % mt], ["ps2"])
                OP("dve", "reciprocal", ["ps2"], ["rden"], out=rden, in_=PS(2))
                for cc in range(2):
                    for mt in range(2):
                        MM(PS(3 + cc), Vm[:, mt, h * 256 + cc * 128:h * 256 + (cc + 1) * 128], PT2[:, mt, :], mt == 0, mt == 1,
                           ["Vm", "PT20", "PT21"], ["ps%d" % (3 + cc)])
                    OP("dve", "tensor_tensor", ["ps%d" % (3 + cc), "rden"], ["oT"], out=oT[:, 2 * h + cc, :], in0=PS(3 + cc),
                       in1=rden, op=ALU.mult)
            for t in range(4):
                tt = G * 4 + t
                s2 = t % 2
                for half in range(2):
                    for k in range(8):
                        MM(PS(half), oT[:, k, t * 128:(t + 1) * 128], w_co[:, k, half * 512:(half + 1) * 512], k == 0, k == 7,
                           ["oT", "w_co"], ["ps%d" % half])
                layer_norm_tile(h1f[:, t, :], PS(0), PS(1), lng, lnb, pre[:, s2, :], h2t[:, s2, :], lsm[:, s2, 0:12],
                                lsm[:, s2, 12:15], lsm[:, s2, 15:16], "l2%d" % s2, ["h1f%d" % t], ["ps0", "ps1"])
                DMA("sp", h2_d[tt * 128:(tt + 1) * 128, :], h2t[:, s2, :], ["l2%dout" % s2], ["h2_d%d" % s2])
                ACT(h2b[:, s2, :], h2t[:, s2, :], AF.Copy, ["l2%dout" % s2], ["h2b%d" % s2])
                if pend_r is not None:
                    router_tile(*pend_r)
                pend_r = (tt, s2)
            if pend_r is not None:
                router_tile(*pend_r)
                pend_r = None
    P.barrier_all()
    if stage == "2":
        DMA("sp", dbg2_d[:, 0:128], g4_all.rearrange("p a b -> p (a b)"), [], ["dbg2"])
        DMA("sp", dbg2_d[:, 128:256].bitcast(I32), slot_all.rearrange("p a b -> p (a b)"), [], ["dbg2"])
        P.emit(final_tiles=["dbg2"])
        return nc

    A.release(keep_mark)
    wg = A.alloc([2, 8, 1024], BF16)
    wl = A.alloc([2, 8, 1024], BF16)
    wd = A.alloc([2, 8, 1024], BF16)
    bdn = A.alloc([2, 1024], BF16, parts=1)
    bgl = A.alloc([8, 64], F32)
    braw = A.alloc([2048], F32, parts=32)
    xtok = A.alloc([2, 5, 1024], BF16)
    XT = A.alloc([2, 8, CAP], BF16)
    actT = A.alloc([8, CAP], BF16)
    glu = A.alloc([2, 512], F32)
    sig = A.alloc([2, 512], F32)
    gsx = A.alloc([2, 512], F32)
    lin1 = A.alloc([2, 512], F32)
    yt = A.alloc([2, 1024], BF16)
    DMA("sp", braw[:, 0:1024], bglu_d, [], ["braw"])
    DMA("sp", braw[:, 1024:2048], blin_d, [], ["braw"])
    for c in range(8):
        MM(PS(0, 32, c0=c * 64), braw[:, c * 128:(c + 1) * 128], ident_f[0:32, 0:32], True, True, ["braw"], ["ps0"])
        MM(PS(0, 32, c0=c * 64 + 32), braw[:, 1024 + c * 128:1024 + (c + 1) * 128], ident_f[0:32, 0:32], True, True,
           ["braw"], ["ps0"])
    OP("dve", "tensor_copy", ["ps0"], ["bgl"], out=bgl, in_=PS(0).rearrange("p (c e) -> p c e", c=8))
    OP("dve", "tensor_scalar", ["bgl"], ["bgl"], out=bgl[:, :, 32:64], in0=bgl[:, :, 32:64], scalar1=1.0, scalar2=None,
       op0=ALU.add)
    TG = ((0, 512), (512, CAP - 512))

    def load_expert(e):
        s2 = e % 2
        for k in range(8):
            DMA("pool", wg[:, s2, k, :], wglu_d[e, k * 128:(k + 1) * 128, :], [], ["wg%d" % s2])
            DMA("pool", wl[:, s2, k, :], wlin_d[e, k * 128:(k + 1) * 128, :], [], ["wl%d" % s2])
            DMA("pool", wd[:, s2, k, :], wdn_d[e, k * 128:(k + 1) * 128, :], [], ["wd%d" % s2])
        DMA("pool", bdn[:, s2, :], bdn_d[e:e + 1, :], [], ["bdn%d" % s2])

    def prep_expert(e):
        x2 = e % 2
        DMA("sp", xtok[:, x2], xs_d[e * CAP:(e + 1) * CAP, :].rearrange("(j p) d -> p j d", p=128), [], ["xtok%d" % x2])
        for j in range(5):
            pb = 6 + j % 2
            for k in range(8):
                OP("pe", "transpose", ["xtok%d" % x2], ["ps%d" % pb], out=PSB(pb, 128, c0=k * 128),
                   in_=xtok[:, x2, j, k * 128:(k + 1) * 128], identity=ident_b)
            OP("dve" if j % 2 == 0 else "act", "tensor_copy" if j % 2 == 0 else "copy", ["ps%d" % pb], ["XT%d" % x2],
               out=XT[:, x2, :, j * 128:(j + 1) * 128], in_=PSB(pb).rearrange("p (k c) -> p k c", k=8))

    load_expert(0)
    for e in range(NE):
        s2 = e % 2
        if e + 1 < NE:
            load_expert(e + 1)
        if e == 0:
            prep_expert(0)
        def gu_tail(f, gi, t0, tn):
            OP("pool", "tensor_tensor", ["glu%d" % gi, "sig%d" % gi], ["gsx%d" % gi], out=gsx[:, gi, 0:tn], in0=glu[:, gi, 0:tn],
               in1=sig[:, gi, 0:tn], op=ALU.mult)
            OP("pool", "tensor_scalar", ["lin%d" % gi], ["lin%d" % gi], out=lin1[:, gi, 0:tn], in0=lin1[:, gi, 0:tn],
               scalar1=8.0, scalar2=-6.0, op0=ALU.min, op1=ALU.max)
            OP("dve", "tensor_tensor", ["lin%d" % gi, "gsx%d" % gi], ["actT"], out=actT[:, f, t0:t0 + tn],
               in0=lin1[:, gi, 0:tn], in1=gsx[:, gi, 0:tn], op=ALU.mult)

        pending = None
        for f in range(8):
            for gi, (t0, tn) in enumerate(TG):
                pg, pl = 0 + gi, 2 + gi
                for k in range(8):
                    MM(PS(pl, tn), wl[:, s2, k, f * 128:(f + 1) * 128], XT[:, s2, k, t0:t0 + tn], k == 0, k == 7,
                       ["wl%d" % s2, "XT%d" % s2], ["ps%d" % pl])
                for k in range(8):
                    MM(PS(pg, tn), wg[:, s2, k, f * 128:(f + 1) * 128], XT[:, s2, k, t0:t0 + tn], k == 0, k == 7,
                       ["wg%d" % s2, "XT%d" % s2], ["ps%d" % pg])
                ACT(lin1[:, gi, 0:tn], PS(pl, tn), AF.Identity, ["ps%d" % pl, "bgl"], ["lin%d" % gi], bias=bgl[:, f, 32 + e:33 + e])
                OP("dve", "tensor_scalar", ["ps%d" % pg, "bgl"], ["glu%d" % gi], out=glu[:, gi, 0:tn], in0=PS(pg, tn),
                   scalar1=bgl[:, f, e:e + 1], scalar2=7.0, op0=ALU.add, op1=ALU.min)
                ACT(sig[:, gi, 0:tn], glu[:, gi, 0:tn], AF.Sigmoid, ["glu%d" % gi], ["sig%d" % gi], scale=1.702)
                if pending is not None:
                    gu_tail(*pending)
                pending = (f, gi, t0, tn)
        gu_tail(*pending)
        if e + 1 < NE:
            prep_expert(e + 1)
        for j in range(5):
            y2 = j % 2
            for half in range(2):
                pb = 4 + half
                for k in range(8):
                    MM(PS(pb), actT[:, k, j * 128:(j + 1) * 128], wd[:, s2, k, half * 512:(half + 1) * 512], k == 0, False,
                       ["actT", "wd%d" % s2], ["ps%d" % pb])
                MM(PS(pb), ones_b[0:1, :], bdn[:, s2, half * 512:(half + 1) * 512], False, True, ["bdn%d" % s2], ["ps%d" % pb])
            ACT(yt[:, y2, 0:512], PS(4), AF.Copy, ["ps4"], ["yt%da" % y2])
            OP("dve", "tensor_copy", ["ps5"], ["yt%db" % y2], out=yt[:, y2, 512:1024], in_=PS(5))
            DMA("sp", ys_d[e * CAP + j * 128:e * CAP + (j + 1) * 128, :], yt[:, y2, :], ["yt%da" % y2, "yt%db" % y2],
                ["ys_w%d" % y2])
    P.barrier_all()

    A.release(keep_mark)
    lng = A.alloc([1024], F32)
    lnb = A.alloc([1024], F32)
    NBUF = 3
    yk = A.alloc([NBUF, 4, 1024], BF16)
    macc = A.alloc([2, 1024], F32)
    h2r = A.alloc([NBUF, 1024], F32)
    pre = A.alloc([2, 1024], F32)
    ot = A.alloc([2, 1024], F32)
    lsm = A.alloc([2, 16], F32)
    DMA("sp", lng, lnp_d[4:5, :].broadcast_to([128, 1024]), [], ["lnp"])
    DMA("sp", lnb, lnp_d[5:6, :].broadcast_to([128, 1024]), [], ["lnp"])
    OP("pool", "memset", [], ["yk%d_%d" % (a_, k4) for a_ in range(NBUF) for k4 in range(4)], yk, 0.0)

    def fetch(tt):
        s3 = tt % NBUF
        DMA("sp", h2r[:, s3, :], h2_d[tt * 128:(tt + 1) * 128, :], [], ["h2r%d" % s3])
        for k4 in range(4):
            P.op("pool", (lambda e, tt=tt, k4=k4, s3=s3: e.indirect_dma_start(
                out=yk[:, s3, k4, :], out_offset=None, in_=ys_d,
                in_offset=bass.IndirectOffsetOnAxis(ap=slot_all[:, tt, k4:k4 + 1], axis=0),
                bounds_check=bc_reg(e), oob_is_err=False)), reads=[], writes=["yk%d_%d" % (s3, k4)], dma=True)

    fetch(0)
    fetch(1)
    for tt in range(T // 128):
        s2 = tt % 2
        s3 = tt % NBUF
        if tt + 2 < T // 128:
            fetch(tt + 2)
        OP("dve", "tensor_scalar", ["yk%d_0" % s3], ["macc%d" % s2], out=macc[:, s2, :], in0=yk[:, s3, 0, :],
           scalar1=g4_all[:, tt, 0:1], scalar2=None, op0=ALU.mult)
        for k4 in range(1, 4):
            OP("dve", "scalar_tensor_tensor", ["yk%d_%d" % (s3, k4), "macc%d" % s2], ["macc%d" % s2], out=macc[:, s2, :],
               in0=yk[:, s3, k4, :], scalar=g4_all[:, tt, k4:k4 + 1], in1=macc[:, s2, :], op0=ALU.mult, op1=ALU.add)
        layer_norm_tile(h2r[:, s3, :], macc[:, s2, 0:512], macc[:, s2, 512:1024], lng, lnb, pre[:, s2, :], ot[:, s2, :],
                        lsm[:, s2, 0:12], lsm[:, s2, 12:15], lsm[:, s2, 15:16], "l3%d" % s2, ["h2r%d" % s3],
                        ["macc%d" % s2, "macc%d" % s2])
        DMA("sp", out_d[tt * 128:(tt + 1) * 128, :], ot[:, s2, :], ["l3%dout" % s2], ["out%d" % s2])
    P.emit(final_tiles=["out0", "out1"])
    return nc


def _host_layout(inp):
    f = lambda a: np.ascontiguousarray(np.asarray(a), dtype=np.float32)
    w_in = f(inp["w_in"])[0]
    w_a = np.concatenate([w_in[:, 0:416], w_in[:, 320:384], w_in[:, 400:416], w_in[:, 384:400]], axis=1)
    w_b = w_in[:, 416:1960]
    w_c = w_in[:, 1960:4008]
    wuq = f(inp["w_uq"])[0].reshape(256, 8, 96)
    wuq_sw = np.concatenate([wuq[:, :, 0:64], wuq[:, :, 80:96], wuq[:, :, 64:80]], axis=2)
    w_uq2 = np.concatenate([wuq.reshape(256, 768), wuq_sw.reshape(256, 768)], axis=1)
    wukv = f(inp["w_ukv"])[0].reshape(128, 8, 128)
    w_ukv2 = np.concatenate([wukv[:, :, 0:64].reshape(128, 512), wukv[:, :, 64:128].reshape(128, 512)], axis=1)
    w_mq = f(inp["w_mq"])[0].transpose(1, 0, 2).reshape(128, 512)
    w_mk = f(inp["w_mk"])[0].transpose(1, 0, 2).reshape(128, 512)
    w_mqk = np.concatenate([w_mq, w_mk], axis=1)
    pm = np.zeros((128, 32), np.float32)
    cw = f(inp["conv_w"])[0]
    pm[:, 0:16] = cw.T.reshape(4, 128, 4).transpose(1, 0, 2).reshape(128, 16)
    pm[:, 16:20] = f(inp["conv_b"])[0].reshape(4, 128).T
    pm[:, 20:24] = f(inp["g_mhead"])[0].reshape(4, 128).T
    pm[:, 24:28] = f(inp["w_mskip"])[0].reshape(4, 128).T
    pm[:, 28:30] = f(inp["g_qlat"])[0].reshape(2, 128).T
    pm[:, 30] = f(inp["g_kvlat"])[0]
    gb = np.concatenate([f(inp["b_igate"])[0], f(inp["b_fgate"])[0]])[None, :]
    lnp = np.stack([f(inp[k])[0] for k in ("ln1_g", "ln1_b", "ln2_g", "ln2_b", "ln3_g", "ln3_b")])
    w_gu = f(inp["w_gu"])[0]
    b_gu = f(inp["b_gu"])[0]
    shared = {
        "w_a": w_a, "w_b": w_b, "w_c": w_c, "w_uq2": w_uq2, "w_ukv2": w_ukv2, "w_mqk": w_mqk, "pm": pm, "gb": gb,
        "lnp": lnp, "w_br_m": f(inp["w_br_m"])[0], "w_br_a": f(inp["w_br_a"])[0], "w_mix": f(inp["w_mix_out"])[0],
        "w_cq": f(inp["w_cq"])[0], "w_ckv": f(inp["w_ckv"])[0], "w_co": f(inp["w_co"])[0],
        "w_router": f(inp["w_router"])[0], "b_router": f(inp["b_router"]),
        "w_glu": w_gu[:, :, 0::2], "w_lin": w_gu[:, :, 1::2], "w_dn": f(inp["w_dn"])[0],
        "b_glu": b_gu[:, 0::2], "b_lin": b_gu[:, 1::2], "b_dn": f(inp["b_dn"])[0],
    }
    shared = {k: np.ascontiguousarray(v, dtype=np.float32) for k, v in shared.items()}
    x = f(inp["x"])
    mem = f(inp["mem"])
    pos = np.ascontiguousarray(np.asarray(inp["positions"]), dtype=np.int32)
    maps = []
    for c in range(N_CORES):
        m = dict(shared)
        m["x"] = np.ascontiguousarray(x[c * NB:(c + 1) * NB].reshape(T, D))
        m["mem"] = np.ascontiguousarray(mem[c * NB:(c + 1) * NB].reshape(NB * 256, D))
        m["pos"] = np.ascontiguousarray(pos[c * NB:(c + 1) * NB])
        maps.append(m)
    return maps


def kernel(**inputs):
    maps = _host_layout(inputs)
    nc = build_program()
    res = run_bass_kernel_spmd(nc, maps, core_ids=list(range(N_CORES)))
    out = np.concatenate([np.asarray(r["out"]).reshape(NB, S, D) for r in res.results], axis=0)
    return out.astype(np.float32)
```
